# Optimizing a Trainium2 kernel written in Bass

```python
import jax, jax.numpy as jnp
from jax import lax
import numpy as np

D_MODEL = 1024
BATCH = 1
SEQ = 16384
DEPTH = 4

D_MIX = D_MODEL
A_WIDTH = D_MIX // 2
A_HEADS = 8
A_HEAD_DIM = A_WIDTH // A_HEADS
A_CHUNK = 128
B_WIDTH = D_MIX - A_WIDTH
CONV_WIDTH = 3
PROJ_AB = 2 * A_WIDTH + 3 * B_WIDTH
C_GROUPS = 4
C_WINDOWS = (2, 4, 8, 16)
C_GROUP_DIM = D_MIX // C_GROUPS
N_EXPERTS = 32
TOP_K = 4
D_FF = D_MODEL
SWIGLU_LIMIT = 7.0
SWIGLU_ALPHA = 1.702
MOE_BLOCK = 256
LN_EPS = 1e-5
DEEPNORM_ALPHA = float((2 * DEPTH) ** 0.25)
DEEPNORM_BETA = float((8 * DEPTH) ** -0.25)
N_EVEN = (DEPTH + 1) // 2
N_ODD = DEPTH // 2

kernel_name = "hybrid_gmlp_shortconv_pool_moe_deepnorm"


def layer_norm(x, g, b=None):
    xf = x.astype(jnp.float32)
    mu = jnp.mean(xf, axis=-1, keepdims=True)
    var = jnp.mean(jnp.square(xf - mu), axis=-1, keepdims=True)
    y = (xf - mu) * lax.rsqrt(var + LN_EPS) * g.astype(jnp.float32)
    if b is not None:
        y = y + b.astype(jnp.float32)
    return y.astype(x.dtype)


def spatial_gating(u, v, norm_g, w_s, b_s):
    bsz, s, _ = v.shape
    v = layer_norm(v, norm_g)
    mask = jnp.tril(jnp.ones((A_CHUNK, A_CHUNK), dtype=bool))
    w = jnp.where(mask, w_s, jnp.zeros((), w_s.dtype))
    vc = v.reshape(bsz, s // A_CHUNK, A_CHUNK, A_HEADS, A_HEAD_DIM)
    sv = jnp.einsum('hts,bnshd->bnthd', w, vc) + b_s.T[None, None, :, :, None]
    return u * sv.reshape(bsz, s, A_WIDTH)


def short_gated_conv(g_b, g_c, x_in, conv_w):
    z = g_c * x_in
    s = z.shape[1]
    zp = jnp.pad(z, ((0, 0), (CONV_WIDTH - 1, 0), (0, 0)))
    conv = conv_w[0] * zp[:, 0:s]
    for k in range(1, CONV_WIDTH):
        conv = conv + conv_w[k] * zp[:, k:k + s]
    return g_b * conv


def multiscale_pool(z, w_grp, scale):
    bsz, s, _ = z.shape
    zf = z.astype(jnp.float32).reshape(bsz, s, C_GROUPS, C_GROUP_DIM)
    cs = jnp.cumsum(zf, axis=1)
    pos = jnp.arange(s)
    outs = []
    for g, win in enumerate(C_WINDOWS):
        csg = cs[:, :, g]
        lag = jnp.pad(csg, ((0, 0), (win, 0), (0, 0)))[:, :s]
        cnt = jnp.minimum(pos + 1, win).astype(jnp.float32)[None, :, None]
        outs.append((csg - lag) / cnt - zf[:, :, g])
    pooled = jnp.stack(outs, axis=2).astype(z.dtype)
    mixed = jnp.einsum('bsgc,gcd->bsgd', pooled, w_grp)
    return mixed.reshape(bsz, s, D_MIX) * scale


def clamped_swiglu(h):
    gate, lin = h[..., :D_FF], h[..., D_FF:]
    gate = jnp.minimum(gate, SWIGLU_LIMIT)
    lin = jnp.clip(lin, -SWIGLU_LIMIT, SWIGLU_LIMIT)
    return (lin + 1.0) * gate * jax.nn.sigmoid(SWIGLU_ALPHA * gate)


def moe_ffn(x, router_w, router_b, w_up, b_up, w_down, b_down):
    bsz, s, d = x.shape
    xt = x.reshape(-1, d)
    n_tok = xt.shape[0]
    logits = (xt @ router_w + router_b).astype(jnp.float32)
    top_val, top_idx = lax.top_k(logits, TOP_K)
    gates = jax.nn.softmax(top_val, axis=-1).astype(x.dtype)

    n_pair = n_tok * TOP_K
    flat_e = top_idx.reshape(-1)
    order = jnp.argsort(flat_e)
    e_sorted = flat_e[order]
    tok_sorted = order // TOP_K
    counts = jnp.bincount(flat_e, length=N_EXPERTS)
    starts = jnp.cumsum(counts) - counts
    padded_counts = (counts + MOE_BLOCK - 1) // MOE_BLOCK * MOE_BLOCK
    padded_ends = jnp.cumsum(padded_counts)
    padded_starts = padded_ends - padded_counts
    dest = padded_starts[e_sorted] + (jnp.arange(n_pair) - starts[e_sorted])
    n_blocks = -(-n_pair // MOE_BLOCK) + N_EXPERTS
    x_pad = jnp.zeros((n_blocks * MOE_BLOCK, d), x.dtype).at[dest].set(xt[tok_sorted])
    block_start = jnp.arange(n_blocks) * MOE_BLOCK
    block_expert = jnp.minimum(
        jnp.searchsorted(padded_ends, block_start, side='right'), N_EXPERTS - 1)

    def expert_block(args):
        xb, e = args
        h = xb @ w_up[e] + b_up[e]
        return clamped_swiglu(h) @ w_down[e] + b_down[e]

    y_pad = lax.map(expert_block, (x_pad.reshape(n_blocks, MOE_BLOCK, d), block_expert))
    y_sorted = y_pad.reshape(-1, d)[dest]
    y_pairs = jnp.zeros_like(y_sorted).at[order].set(y_sorted).reshape(n_tok, TOP_K, d)
    return jnp.einsum('tkd,tk->td', y_pairs, gates).reshape(bsz, s, d)


def setup_inputs(seed: int = 0) -> dict:
    key = jax.random.key(seed)
    ks = jax.random.split(key, 24)
    f32 = jnp.float32
    nrm = lambda k, shape, scale: jax.random.normal(k, shape, f32) * scale
    d_in = D_MODEL ** -0.5
    return {
        "x": nrm(ks[0], (BATCH, SEQ, D_MODEL), 1.0),
        "ab_w_in": nrm(ks[1], (N_EVEN, D_MODEL, PROJ_AB), d_in),
        "a_norm_g": 1.0 + nrm(ks[2], (N_EVEN, A_WIDTH), 0.05),
        "a_w_s": nrm(ks[3], (N_EVEN, A_HEADS, A_CHUNK, A_CHUNK), A_CHUNK ** -0.5),
        "a_b_s": 1.0 + nrm(ks[4], (N_EVEN, A_HEADS, A_CHUNK), 0.05),
        "b_conv_w": nrm(ks[5], (N_EVEN, CONV_WIDTH, B_WIDTH), CONV_WIDTH ** -0.5),
        "ab_w_out": nrm(ks[6], (N_EVEN, D_MIX, D_MODEL), D_MIX ** -0.5 * DEEPNORM_BETA),
        "c_w_in": nrm(ks[7], (N_ODD, D_MODEL, D_MIX), d_in),
        "c_w_grp": nrm(ks[8], (N_ODD, C_GROUPS, C_GROUP_DIM, C_GROUP_DIM), C_GROUP_DIM ** -0.5),
        "c_scale": 1.0 + nrm(ks[9], (N_ODD, D_MIX), 0.05),
        "c_w_out": nrm(ks[10], (N_ODD, D_MIX, D_MODEL), D_MIX ** -0.5 * DEEPNORM_BETA),
        "ln_mix_g": 1.0 + nrm(ks[11], (DEPTH, D_MODEL), 0.05),
        "ln_mix_b": nrm(ks[12], (DEPTH, D_MODEL), 0.02),
        "router_w": nrm(ks[13], (DEPTH, D_MODEL, N_EXPERTS), d_in),
        "router_b": nrm(ks[14], (DEPTH, N_EXPERTS), 0.01),
        "moe_w_up": nrm(ks[15], (DEPTH, N_EXPERTS, D_MODEL, 2 * D_FF), d_in),
        "moe_b_up": nrm(ks[16], (DEPTH, N_EXPERTS, 2 * D_FF), 0.02),
        "moe_w_down": nrm(ks[17], (DEPTH, N_EXPERTS, D_FF, D_MODEL), D_FF ** -0.5 * DEEPNORM_BETA),
        "moe_b_down": nrm(ks[18], (DEPTH, N_EXPERTS, D_MODEL), 0.02),
        "ln_ffn_g": 1.0 + nrm(ks[19], (DEPTH, D_MODEL), 0.05),
        "ln_ffn_b": nrm(ks[20], (DEPTH, D_MODEL), 0.02),
    }


def reference(x, ab_w_in, a_norm_g, a_w_s, a_b_s, b_conv_w, ab_w_out,
              c_w_in, c_w_grp, c_scale, c_w_out, ln_mix_g, ln_mix_b,
              router_w, router_b, moe_w_up, moe_b_up, moe_w_down, moe_b_down,
              ln_ffn_g, ln_ffn_b):
    splits = [A_WIDTH, 2 * A_WIDTH, 2 * A_WIDTH + B_WIDTH, 2 * A_WIDTH + 2 * B_WIDTH]
    for layer in range(DEPTH):
        i = layer // 2
        if layer % 2 == 0:
            z = x @ ab_w_in[i]
            u, v, g_b, g_c, x_in = jnp.split(z, splits, axis=-1)
            y_a = spatial_gating(jax.nn.gelu(u, approximate=False),
                                 jax.nn.gelu(v, approximate=False),
                                 a_norm_g[i], a_w_s[i], a_b_s[i])
            y_b = short_gated_conv(g_b, g_c, x_in, b_conv_w[i])
            mix = jnp.concatenate([y_a, y_b], axis=-1) @ ab_w_out[i]
        else:
            mix = multiscale_pool(x @ c_w_in[i], c_w_grp[i], c_scale[i]) @ c_w_out[i]
        x = layer_norm(DEEPNORM_ALPHA * x + mix, ln_mix_g[layer], ln_mix_b[layer])
        ffn = moe_ffn(x, router_w[layer], router_b[layer], moe_w_up[layer],
                      moe_b_up[layer], moe_w_down[layer], moe_b_down[layer])
        x = layer_norm(DEEPNORM_ALPHA * x + ffn, ln_ffn_g[layer], ln_ffn_b[layer])
    return x
```

```python
import contextlib
import numpy as np
import ml_dtypes
import concourse.bass as bass
import concourse.mybir as mybir
from concourse.bass_utils import run_bass_kernel_spmd

F32 = mybir.dt.float32
BF16 = mybir.dt.bfloat16
U32 = mybir.dt.uint32
ALU = mybir.AluOpType
AF = mybir.ActivationFunctionType

NCORES = 8
D = 1024
SEQ = 16384
TOK = SEQ // NCORES
HALO = 256
T = TOK + HALO
NTL = T // 128
NE = 32
TOPK = 4
EPC = NE // NCORES
C = 384
NSLOT = NE * C
DUMP = NSLOT
XS_ROWS = NSLOT + 128
ALPHA = float(8 ** 0.25)
EPS = 1e-5
GT = 3
NT = GT * 128
DEPTH = 4

ENGS = ("pe", "dve", "act", "pool", "sp")


class Buf:
    __slots__ = ("name", "w", "rs")

    def __init__(self, name):
        self.name = name
        self.w = None
        self.rs = []


class Sched:
    def __init__(self, nc, stack):
        self.nc = nc
        self.stack = stack
        self.streams = {e: [] for e in ENGS}
        self.esem = {e: stack.enter_context(nc.semaphore("s_" + e)) for e in ENGS}
        self.ecnt = {e: 0 for e in ENGS}
        self.waited = {e: {} for e in ENGS}
        self.dsem = {}
        self.bg = None
        self._in_bg = False

    def _tick(self):
        if self.bg is not None and not self._in_bg:
            self._in_bg = True
            try:
                next(self.bg)
            except StopIteration:
                self.bg = None
            self._in_bg = False

    def drain(self):
        while self.bg is not None:
            self._tick()

    def _deps(self, eng, reads, writes):
        deps = []
        for b in reads:
            if b.w is not None:
                deps.append(b.w)
        for b in writes:
            if b.w is not None and b.w[0] != eng:
                deps.append(b.w)
            for r in b.rs:
                if r[0] != eng:
                    deps.append(r)
        need = {}
        for (e, sem, val) in deps:
            k = id(sem)
            if k not in need or need[k][1] < val:
                need[k] = (sem, val)
        out = []
        w = self.waited[eng]
        for k, (sem, val) in need.items():
            if w.get(k, 0) < val:
                w[k] = val
                out.append((sem, val))
        return out

    def _commit(self, tok, reads, writes):
        for b in writes:
            b.w = tok
            b.rs = []
        for b in reads:
            b.rs.append(tok)
            if len(b.rs) > 16:
                latest = {}
                for r in b.rs:
                    k = id(r[1])
                    if k not in latest or latest[k][2] < r[2]:
                        latest[k] = r
                b.rs = list(latest.values())

    def op(self, eng, fn, reads=(), writes=()):
        waits = self._deps(eng, reads, writes)
        self.ecnt[eng] += 1
        sem = self.esem[eng]
        tok = (eng, sem, self.ecnt[eng])

        def run(E, waits=waits, fn=fn, sem=sem):
            for (s, v) in waits:
                E.wait_ge(s, v)
            fn(E).then_inc(sem, 1)
        self.streams[eng].append(run)
        self._commit(tok, reads, writes)
        self._tick()
        return tok

    def op_nosig(self, eng, fn, reads=(), writes=()):
        waits = self._deps(eng, reads, writes)

        def run(E, waits=waits, fn=fn):
            for (s, v) in waits:
                E.wait_ge(s, v)
            fn(E)
        self.streams[eng].append(run)

    def dma(self, qeng, fn, key, reads=(), writes=()):
        if key not in self.dsem:
            self.dsem[key] = [self.stack.enter_context(self.nc.semaphore("d_" + key)), 0]
        ent = self.dsem[key]
        sem = ent[0]
        waits = self._deps(qeng, reads, writes)
        if ent[1] > 0 and self.waited[qeng].get(id(sem), 0) < ent[1]:
            self.waited[qeng][id(sem)] = ent[1]
            waits = waits + [(sem, ent[1])]
        ent[1] += 16
        tok = ("dma:" + key, sem, ent[1])

        def run(E, waits=waits, fn=fn, sem=sem):
            for (s, v) in waits:
                E.wait_ge(s, v)
            fn(E).then_inc(sem, 16)
        self.streams[qeng].append(run)
        self._commit(tok, reads, writes)
        self._tick()
        return tok

    def barrier(self):
        self.drain()
        targets = [(self.esem[e], self.ecnt[e]) for e in ENGS if self.ecnt[e] > 0]
        targets += [(ent[0], ent[1]) for ent in self.dsem.values() if ent[1] > 0]
        for e in ENGS:
            w = self.waited[e]
            mine = []
            for (s, v) in targets:
                if s is self.esem[e]:
                    continue
                if w.get(id(s), 0) < v:
                    w[id(s)] = v
                    mine.append((s, v))

            def run(E, mine=mine):
                for (s, v) in mine:
                    E.wait_ge(s, v)
            self.streams[e].append(run)

    def emit(self):
        nc = self.nc
        with nc.Block() as block:
            @block.tensor
            def _(E):
                for f in self.streams["pe"]:
                    f(E)

            @block.vector
            def _(E):
                for f in self.streams["dve"]:
                    f(E)

            @block.scalar
            def _(E):
                for f in self.streams["act"]:
                    f(E)

            @block.gpsimd
            def _(E):
                for f in self.streams["pool"]:
                    f(E)

            @block.sync
            def _(E):
                for f in self.streams["sp"]:
                    f(E)
        self.streams = {e: [] for e in ENGS}


class Ring:
    def __init__(self, alloc, name, shape, dt, n):
        self.t = [alloc("%s%d" % (name, i), shape, dt) for i in range(n)]
        self.b = [Buf("%s%d" % (name, i)) for i in range(n)]
        self.i = 0

    def get(self):
        r = (self.t[self.i], self.b[self.i])
        self.i = (self.i + 1) % len(self.t)
        return r


class Ctx:
    def __init__(self, name):
        self.nc = bass.Bass("TRN2", target_bir_lowering=False)
        self.stack = contextlib.ExitStack()
        self.S = Sched(self.nc, self.stack)
        self.pstack = self.stack
        self.phase = 0
        self.nck = 0

    def dram_in(self, name, shape, dt):
        return self.nc.dram_tensor(name, list(shape), dt, kind="ExternalInput").ap()

    def dram_out(self, name, shape, dt):
        return self.nc.dram_tensor(name, list(shape), dt, kind="ExternalOutput").ap()

    def dram_tmp(self, name, shape, dt):
        return self.nc.dram_tensor(name, list(shape), dt).ap()

    def sb(self, name, shape, dt):
        return self.pstack.enter_context(self.nc.sbuf_tensor("sb%d_%s" % (self.phase, name), list(shape), dt))

    def ps(self, name, shape, dt):
        return self.pstack.enter_context(self.nc.psum_tensor("ps%d_%s" % (self.phase, name), list(shape), dt))

    def ckey(self):
        self.nck += 1
        return "const%d" % (self.nck % 6)

    def ldq(self):
        return "sp"

    def begin_phase(self):
        self.phase += 1
        self.pstack = contextlib.ExitStack()

    def end_phase(self):
        self.S.barrier()
        self.S.emit()
        self.pstack.close()
        self.pstack = self.stack

    def finish(self):
        self.S.barrier()
        self.S.emit()
        self.stack.close()
        return self.nc


def mm_chain(S, ps_ap, ps_buf, pairs, reads):
    n = len(pairs)
    for i, (l, r) in enumerate(pairs):
        fn = (lambda E, l=l, r=r, i=i: E.matmul(ps_ap, lhsT=l, rhs=r, start=(i == 0), stop=(i == n - 1)))
        if i < n - 1:
            S.op_nosig("pe", fn, reads=reads if i == 0 else (), writes=[ps_buf] if i == 0 else ())
        else:
            return S.op("pe", fn, reads=reads, writes=[ps_buf])


def load_consts(K, need_router=True):
    S = K.S
    cs = {}
    specs = [("ident_bf", [128, 128], BF16), ("ident_f", [128, 128], F32)]
    if need_router:
        specs += [("triu", [128, 128], BF16), ("ones_bf", [128, 128], BF16),
                  ("tril", [128, 128], F32), ("iota", [128, NE], F32), ("dump_p", [128, 1], F32)]
    for (nm, shp, dt) in specs:
        src = K.dram_in(nm, shp, dt)
        t = K.sb("c_" + nm, shp, dt)
        b = Buf("c_" + nm)
        S.dma("sp", lambda E, t=t, src=src: E.dma_start(out=t[:], in_=src), K.ckey(), writes=[b])
        cs[nm] = (t, b)
    return cs


def bcast_load(K, name, src_row, n, key=None):
    t = K.sb(name, [128, n], F32)
    b = Buf(name)
    key = key or K.ckey()
    K.S.dma("sp", lambda E: E.dma_start(out=t[:], in_=src_row.partition_broadcast(128)), key, writes=[b])
    return t, b


def layer_norm_tile(K, h, hB, g, gB, bta, bB, msk_ap, mB, out, outB, st):
    S = K.S
    stats, stB = st["stats"].get()
    mv, mvB = st["mv"].get()
    rstd, rsB = st["rstd"].get()
    for hh in range(2):
        S.op("dve", lambda E, hh=hh: E.bn_stats(out=stats[:, hh, :], in_=h[:, hh * 512:(hh + 1) * 512]),
             reads=[hB], writes=[stB])
    S.op("dve", lambda E: E.bn_aggr(out=mv[:], in_=stats[:].rearrange("p a b -> p (a b)")), reads=[stB], writes=[mvB])
    S.op("act", lambda E: E.activation(out=rstd[:, 0:1], in_=mv[:, 1:2], func=AF.Sqrt, bias=st["eps"][0][:, 0:1], scale=1.0),
         reads=[mvB, st["eps"][1]], writes=[rsB])
    S.op("dve", lambda E: E.reciprocal(out=rstd[:, 1:2], in_=rstd[:, 0:1]), reads=[rsB], writes=[rsB])
    if msk_ap is not None:
        S.op("dve", lambda E: E.tensor_tensor(out=rstd[:, 2:3], in0=rstd[:, 1:2], in1=msk_ap, op=ALU.mult),
             reads=[rsB, mB], writes=[rsB])
        rs_ap = rstd[:, 2:3]
    else:
        rs_ap = rstd[:, 1:2]
    t1, t1B = st["t4k"].get()
    S.op("dve", lambda E: E.tensor_scalar(out=t1[:], in0=h[:], scalar1=mv[:, 0:1], scalar2=rs_ap,
                                          op0=ALU.subtract, op1=ALU.mult), reads=[hB, mvB, rsB], writes=[t1B])
    S.op("dve", lambda E: E.tensor_tensor(out=t1[:], in0=t1[:], in1=g[:], op=ALU.mult), reads=[t1B, gB], writes=[t1B])
    if msk_ap is not None:
        S.op("dve", lambda E: E.scalar_tensor_tensor(out=out[:], in0=bta[:], scalar=msk_ap, in1=t1[:],
                                                     op0=ALU.mult, op1=ALU.add), reads=[bB, mB, t1B], writes=[outB])
    else:
        S.op("dve", lambda E: E.tensor_tensor(out=out[:], in0=t1[:], in1=bta[:], op=ALU.add), reads=[t1B, bB], writes=[outB])


def make_ln_state(K):
    st = {
        "stats": Ring(K.sb, "ln_stats", [128, 2, 6], F32, 2),
        "mv": Ring(K.sb, "ln_mv", [128, 2], F32, 2),
        "rstd": Ring(K.sb, "ln_rstd", [128, 4], F32, 2),
        "t4k": Ring(K.sb, "ln_t", [128, D], F32, 2),
    }
    eps = K.sb("ln_eps", [128, 1], F32)
    eB = Buf("ln_eps")
    K.S.op("pool", lambda E: E.memset(eps[:], EPS), writes=[eB])
    st["eps"] = (eps, eB)
    return st


def combine_phase(K, cs, lnst, x1p, yr, destp, gatep, lng_row, lnb_row, tmask, out_fn, big):
    S = K.S
    g, gB = bcast_load(K, "lnf_g", lng_row, D)
    bta, bB = bcast_load(K, "lnf_b", lnb_row, D)
    dst = K.sb("cmb_dest", [128, NTL * TOPK], U32)
    dB = Buf("cmb_dest")
    gt = K.sb("cmb_gate", [128, NTL * TOPK], F32)
    gtB = Buf("cmb_gate")
    S.dma("sp", lambda E: E.dma_start(out=dst[:], in_=destp), K.ckey(), writes=[dB])
    S.dma("sp", lambda E: E.dma_start(out=gt[:], in_=gatep), K.ckey(), writes=[gtB])
    outB = Buf("cmb_out_dram")
    yrB = Buf("yr_dram")
    for j in range(NTL):
        dest_ap = out_fn(j)
        if dest_ap is None:
            continue
        xt, xB = big.get()
        S.dma("sp", lambda E, xt=xt, j=j: E.dma_start(out=xt[:], in_=x1p[j * 128:(j + 1) * 128, :]), "cmb_x%d" % (j % 2), writes=[xB])
        acc, aB = big.get()
        S.op("dve", lambda E, acc=acc, xt=xt: E.tensor_scalar(out=acc[:], in0=xt[:], scalar1=ALPHA, scalar2=None, op0=ALU.mult),
             reads=[xB], writes=[aB])
        for k in range(TOPK):
            yk, yB = big.get()
            col = j * TOPK + k
            S.dma("pool", lambda E, yk=yk, col=col: E.indirect_dma_start(
                out=yk[:], out_offset=None, in_=yr,
                in_offset=bass.IndirectOffsetOnAxis(ap=dst[:, col:col + 1], axis=0)),
                "cmb_g%d" % k, reads=[dB, yrB], writes=[yB])
            S.op("dve", lambda E, yk=yk, acc=acc, col=col: E.scalar_tensor_tensor(
                out=acc[:], in0=yk[:], scalar=gt[:, col:col + 1], in1=acc[:], op0=ALU.mult, op1=ALU.add),
                reads=[yB, gtB, aB], writes=[aB])
        o, oB = big.get()
        layer_norm_tile(K, acc, aB, g, gB, bta, bB, tmask[0][:, j:j + 1], tmask[1], o, oB, lnst)
        S.dma("act", lambda E, o=o, dest_ap=dest_ap: E.dma_start(out=dest_ap, in_=o[:]), "cmb_o%d" % (j % 2), reads=[oB], writes=[outB])
    return outB


def emit_A(K, layer, first, io, cs, lnst, tm, tmB):
    even = (layer % 2 == 0)
    S = K.S
    nc = K.nc
    idb, idbB = cs["ident_bf"]
    idf, idfB = cs["ident_f"]
    big = Ring(K.sb, "big", [128, D], F32, 12)

    if first:
        x_in = io["x"]
        xin_B = Buf("x_in")
    else:
        x1p = io["x1p"]
        yr = io["yr"]
        destp = io["destp"]
        gatep = io["gatep"]
        lnfg = io["lnf_g_prev"]
        lnfb = io["lnf_b_prev"]
        x_in = io["x_mid"]
        xin_B = combine_phase(K, cs, lnst, x1p, yr, destp, gatep, lnfg, lnfb, (tm, tmB),
                              lambda j: x_in[j * 128:(j + 1) * 128, :], big)

    xs = io["xs"]
    x1_out = io["x1"]
    dest_out = io["dest"]
    gate_out = io["gate"]

    lnm_g, lnm_gB = bcast_load(K, "lnm_g", io["lnm_g"], D)
    lnm_b, lnm_bB = bcast_load(K, "lnm_b", io["lnm_b"], D)
    rw_d = io["router_w"]
    rw = K.sb("rw", [128, 8, NE], F32)
    rwB = Buf("rw")
    S.dma("sp", lambda E: E.dma_start(out=rw[:], in_=rw_d.rearrange("(k p) e -> p k e", p=128)), K.ckey(), writes=[rwB])
    rb, rbB = bcast_load(K, "rb", io["router_b"], NE)

    wout_d = io["w_out"]
    wout = K.sb("wout", [128, 8, D], BF16)
    woutB = Buf("wout")
    S.dma("pool", lambda E: E.dma_start(out=wout[:], in_=wout_d.rearrange("(k p) n -> p k n", p=128)), "wld", writes=[woutB])
    if even:
        win_d = io["w_in"]
        win = K.sb("win", [128, 8, 2560], BF16)
        winB = Buf("win")
        for hh in range(2):
            S.dma("pool", lambda E, hh=hh: E.dma_start(out=win[:, hh * 4:(hh + 1) * 4, :],
                                                      in_=win_d.rearrange("(k p) n -> p k n", p=128)[:, hh * 4:(hh + 1) * 4, :]),
                  "wld", writes=[winB])
        ang, angB = bcast_load(K, "a_norm_g", io["a_norm_g"], 512)
        ws_d = io["a_w_s"]
        wsf = K.sb("wsf", [128, 8, 128], F32)
        wsfB = Buf("wsf")
        S.dma("sp", lambda E: E.dma_start(out=wsf[:], in_=ws_d.rearrange("h t s -> t h s")), K.ckey(), writes=[wsfB])
        wsm = K.sb("wsm", [128, 8, 128], BF16)
        wsmB = Buf("wsm")
        tril, trilB = cs["tril"]
        for h in range(8):
            S.op("dve", lambda E, h=h: E.tensor_tensor(out=wsm[:, h, :], in0=wsf[:, h, :], in1=tril[:], op=ALU.mult),
                 reads=[wsfB, trilB], writes=[wsmB])
        wsT = K.sb("wsT", [128, 8, 128], BF16)
        wsTB = Buf("wsT")
        bs_d = io["a_b_s"]
        bsT = K.sb("bsT", [128, 4, 128], F32)
        bsTB = Buf("bsT")
        for h in range(8):
            c, half = h // 2, h % 2
            S.dma("sp", lambda E, h=h, c=c, half=half: E.dma_start(
                out=bsT[half * 64:(half + 1) * 64, c, :], in_=bs_d[h, :].partition_broadcast(64)), K.ckey(), writes=[bsTB])
        cw_d = io["b_conv_w"]
        cw = K.sb("cw", [128, 4, 3], F32)
        cwB = Buf("cw")
        for kk in range(3):
            for c in range(4):
                S.dma("sp", lambda E, kk=kk, c=c: E.dma_start(
                    out=cw[:, c, kk:kk + 1], in_=cw_d[kk, c * 128:(c + 1) * 128].rearrange("(p o) -> p o", o=1)),
                    K.ckey(), writes=[cwB])
    else:
        cin_d = io["w_in"]
        cin = K.sb("cin", [128, 8, D], BF16)
        cinB = Buf("cin")
        S.dma("pool", lambda E: E.dma_start(out=cin[:], in_=cin_d.rearrange("(k p) n -> p k n", p=128)), "wld", writes=[cinB])
        wg_d = io["c_w_grp"]
        wg = K.sb("wg", [128, 4, 2, 256], BF16)
        wgB = Buf("wg")
        for gi in range(4):
            S.dma("pool", lambda E, gi=gi: E.dma_start(out=wg[:, gi, :, :], in_=wg_d[gi].rearrange("(cc p) d -> p cc d", p=128)),
                  "wld", writes=[wgB])
        csc_d = io["c_scale"]
        csc = K.sb("csc", [128, 8], F32)
        cscB = Buf("csc")
        for c in range(8):
            S.dma("sp", lambda E, c=c: E.dma_start(out=csc[:, c:c + 1], in_=csc_d[c * 128:(c + 1) * 128].rearrange("(p o) -> p o", o=1)),
                  K.ckey(), writes=[cscB])
        invc_d = io["invc"]

    pT = K.ps("pT", [128, 8, 128], BF16)
    pTB = Buf("pT")
    pF = K.ps("pF", [128, 8, 128], F32)
    pFB = Buf("pF")
    pacc = Ring(K.ps, "pacc", [128, 512], F32, 4)
    prt = K.ps("prt", [128, 512], F32)
    prtB = Buf("prt")

    if even:
        for h in range(8):
            S.op("pe", lambda E, h=h: E.transpose(out=pT[:, h, :], in_=wsm[:, h, :], identity=idb[:]),
                 reads=[wsmB, idbB], writes=[pTB])
        S.op("dve", lambda E: E.tensor_copy(out=wsT[:], in_=pT[:]), reads=[pTB], writes=[wsTB])

    xb_r = Ring(K.sb, "xb", [128, D], BF16, 2)
    xT = K.sb("xT", [128, 8, NT], BF16)
    xTB = Buf("xT")
    yT = K.sb("yT", [128, 8, NT], BF16)
    yTB = Buf("yT")
    sm512 = Ring(K.sb, "sm512", [128, 512], F32, 3)
    fm = Ring(K.sb, "fm", [128, NT], F32, 6)
    if even:
        ug = K.sb("ug", [128, 4, NT], F32)
        ugB = [Buf("ug%d" % c) for c in range(4)]
        vnb_r = Ring(K.sb, "vnb", [128, 512], BF16, 2)
        zc = K.sb("zc", [128, 4, NT + 2], F32)
        zcB = [Buf("zc%d" % c) for c in range(4)]
        for c in range(4):
            S.op("pool", lambda E, c=c: E.memset(zc[:, c, 0:2], 0.0), writes=[zcB[c]])
        sv128 = Ring(K.sb, "sv128", [128, 128], F32, 3)
    else:
        ze = K.sb("ze", [128, 8, NT + 15], F32)
        zeB = [Buf("ze%d" % c) for c in range(8)]
        for c in range(8):
            S.op("pool", lambda E, c=c: E.memset(ze[:, c, 0:15], 0.0), writes=[zeB[c]])
        sAB = Ring(K.sb, "sAB", [128, NT + 15], F32, 3)
        pooled = K.sb("pooled", [128, 8, NT], BF16)
        pooledB = [Buf("pooled%d" % c) for c in range(8)]
        invc_r = Ring(K.sb, "invc", [128, 4, NT], F32, 2)
    x1b_r = Ring(K.sb, "x1b", [128, D], BF16, 3)
    x1T_r = Ring(K.sb, "x1T", [128, 8, 128], F32, 1)
    lg_r = Ring(K.sb, "lg", [128, NE], F32, 2)
    mx_r = Ring(K.sb, "mx", [128, 8], F32, 2)
    mi_r = Ring(K.sb, "mi", [128, 8], U32, 2)
    sm_r = Ring(K.sb, "smalls", [128, 16], F32, 2)
    cr_r = Ring(K.sb, "carry", [128, 16], F32, 2)
    junk_r = Ring(K.sb, "junk", [128, NE], F32, 2)
    Mb_r = Ring(K.sb, "Mb", [128, NE], BF16, 2)
    macc = K.sb("macc", [128, NE], BF16)
    maccB = Buf("macc")
    S.op("pool", lambda E: E.memset(macc[:], 0.0), writes=[maccB])
    dest_all = K.sb("dest_all", [128, NTL * TOPK], U32)
    destB = Buf("dest_all")
    gate_all = K.sb("gate_all", [128, NTL * TOPK], F32)
    gateB = Buf("gate_all")
    xsB = Buf("xs_dram")
    x1oB = Buf("x1_out")
    triu, triuB = cs["triu"]
    ones, onesB = cs["ones_bf"]
    iota, iotaB = cs["iota"]
    dump_p, dump_pB = cs["dump_p"]

    def route_tile(j, x1, x1B):
        x1b, x1bB = x1b_r.get()
        yield
        S.op("act", lambda E: E.copy(out=x1b[:], in_=x1[:]), reads=[x1B], writes=[x1bB])
        for k in range(8):
            yield
            S.op("pe", lambda E, k=k: E.transpose(out=pF[:, k, :], in_=x1[:, k * 128:(k + 1) * 128], identity=idf[:]),
                 reads=[x1B, idfB], writes=[pFB])
        x1T, x1TB = x1T_r.get()
        yield
        S.op("dve", lambda E: E.tensor_copy(out=x1T[:, 0:4, :], in_=pF[:, 0:4, :]), reads=[pFB], writes=[x1TB])
        yield
        S.op("act", lambda E: E.copy(out=x1T[:, 4:8, :], in_=pF[:, 4:8, :]), reads=[pFB], writes=[x1TB])
        pl, plB = prt, prtB
        yield
        mm_chain(S, pl[:, 0:NE], plB, [(x1T[:, k, :], rw[:, k, :]) for k in range(8)], reads=[x1TB, rwB])
        lg, lgB = lg_r.get()
        yield
        S.op("dve", lambda E: E.tensor_tensor(out=lg[:], in0=pl[:, 0:NE], in1=rb[:], op=ALU.add), reads=[plB, rbB], writes=[lgB])
        mx, mxB = mx_r.get()
        mi, miB = mi_r.get()
        yield
        S.op("dve", lambda E: E.max(out=mx[:], in_=lg[:]), reads=[lgB], writes=[mxB])
        yield
        S.op("dve", lambda E: E.max_index(out=mi[:], in_max=mx[:], in_values=lg[:]), reads=[lgB, mxB], writes=[miB])
        sm, smB = sm_r.get()
        yield
        S.op("dve", lambda E: E.tensor_scalar(out=sm[:, 0:1], in0=mx[:, 0:1], scalar1=-1.0, scalar2=None, op0=ALU.mult),
             reads=[mxB], writes=[smB])
        yield
        S.op("act", lambda E: E.activation(out=sm[:, 3:7], in_=mx[:, 0:4], func=AF.Exp, bias=sm[:, 0:1], scale=1.0,
                                           accum_out=sm[:, 1:2]), reads=[mxB, smB], writes=[smB])
        yield
        S.op("dve", lambda E: E.reciprocal(out=sm[:, 2:3], in_=sm[:, 1:2]), reads=[smB], writes=[smB])
        Mb, MbB = Mb_r.get()
        yield
        S.op("dve", lambda E: E.tensor_scalar(out=Mb[:], in0=lg[:], scalar1=mx[:, 3:4], scalar2=tm[:, j:j + 1],
                                              op0=ALU.is_ge, op1=ALU.mult), reads=[lgB, mxB, tmB], writes=[MbB])
        pr, prB = prt[:, NE:2 * NE], prtB
        yield
        mm_chain(S, pr[:, 0:NE], prB, [(triu[:], Mb[:]), (ones[:], macc[:])], reads=[triuB, onesB, MbB, maccB])
        yield
        S.op("dve", lambda E: E.tensor_tensor(out=macc[:], in0=macc[:], in1=Mb[:], op=ALU.add), reads=[maccB, MbB], writes=[maccB])
        yield
        S.op("dve", lambda E: E.tensor_copy(out=sm[:, 7:11], in_=mi[:, 0:4]), reads=[miB], writes=[smB])
        junk, junkB = junk_r.get()
        for k in range(TOPK):
            yield
            S.op("dve", lambda E, k=k: E.scalar_tensor_tensor(
                out=junk[:], in0=iota[:], scalar=sm[:, 7 + k:8 + k], in1=pr[:, 0:NE], op0=ALU.is_equal, op1=ALU.mult,
                accum_out=sm[:, 11 + k:12 + k]), reads=[iotaB, smB, prB], writes=[junkB, smB])
        sm2, sm2B = sm_r.get()
        yield
        S.op("dve", lambda E: E.scalar_tensor_tensor(out=sm2[:, 0:4], in0=sm[:, 7:11], scalar=float(C), in1=sm[:, 11:15],
                                                     op0=ALU.mult, op1=ALU.add), reads=[smB], writes=[sm2B])
        yield
        S.op("dve", lambda E: E.tensor_scalar(out=sm2[:, 4:8], in0=sm[:, 11:15], scalar1=float(C), scalar2=tm[:, j:j + 1],
                                              op0=ALU.is_lt, op1=ALU.mult), reads=[smB, tmB], writes=[sm2B])
        yield
        S.op("dve", lambda E: E.scalar_tensor_tensor(out=sm2[:, 8:12], in0=sm2[:, 0:4], scalar=dump_p[:, 0:1], in1=sm2[:, 4:8],
                                                     op0=ALU.subtract, op1=ALU.mult), reads=[sm2B, dump_pB], writes=[sm2B])
        yield
        S.op("dve", lambda E: E.tensor_scalar(out=sm2[:, 0:4], in0=sm2[:, 8:12], scalar1=dump_p[:, 0:1], scalar2=None, op0=ALU.add),
             reads=[sm2B, dump_pB], writes=[sm2B])
        yield
        S.op("dve", lambda E: E.tensor_copy(out=dest_all[:, j * TOPK:(j + 1) * TOPK], in_=sm2[:, 0:4]), reads=[sm2B], writes=[destB])
        yield
        S.op("dve", lambda E: E.scalar_tensor_tensor(out=gate_all[:, j * TOPK:(j + 1) * TOPK], in0=sm[:, 3:7], scalar=sm[:, 2:3],
                                                     in1=sm2[:, 4:8], op0=ALU.mult, op1=ALU.mult), reads=[smB, sm2B], writes=[gateB])
        for k in range(TOPK):
            col = j * TOPK + k
            yield
            S.dma("pool", lambda E, col=col, x1b=x1b: E.indirect_dma_start(
                out=xs, out_offset=bass.IndirectOffsetOnAxis(ap=dest_all[:, col:col + 1], axis=0), in_=x1b[:], in_offset=None),
                "scat%d" % k, reads=[x1bB, destB], writes=[xsB])

    def post_mix_tile(j, jj, xt, xtB, wbuf, wB):
        h, hB = big.get()
        for n in range(2):
            pm, pmB = pacc.get()
            mm_chain(S, pm[:], pmB, [(yT[:, c, jj * 128:(jj + 1) * 128], wbuf[:, c, n * 512:(n + 1) * 512]) for c in range(8)],
                     reads=[yTB, wB])
            S.op("dve", lambda E, n=n, pm=pm: E.scalar_tensor_tensor(
                out=h[:, n * 512:(n + 1) * 512], in0=xt[:, n * 512:(n + 1) * 512], scalar=ALPHA, in1=pm[:],
                op0=ALU.mult, op1=ALU.add), reads=[xtB, pmB], writes=[hB])
        x1, x1B = big.get()
        layer_norm_tile(K, h, hB, lnm_g, lnm_gB, lnm_b, lnm_bB, tm[:, j:j + 1], tmB, x1, x1B, lnst)
        S.dma("pool", lambda E, x1=x1, j=j: E.dma_start(out=x1_out[j * 128:(j + 1) * 128, :], in_=x1[:]), "x1o", reads=[x1B], writes=[x1oB])
        S.drain()
        S.bg = route_tile(j, x1, x1B)

    for g0 in range(0, NTL, GT):
        xts = []
        for jj in range(GT):
            j = g0 + jj
            xt, xtB = big.get()
            S.dma("sp", lambda E, xt=xt, j=j: E.dma_start(out=xt[:], in_=x_in[j * 128:(j + 1) * 128, :]), "xld%d" % jj,
                  reads=[xin_B], writes=[xtB])
            xts.append((xt, xtB))
            xb, xbB = xb_r.get()
            S.op("act", lambda E, xb=xb, xt=xt: E.copy(out=xb[:], in_=xt[:]), reads=[xtB], writes=[xbB])
            for k in range(8):
                S.op("pe", lambda E, k=k, xb=xb: E.transpose(out=pT[:, k, :], in_=xb[:, k * 128:(k + 1) * 128], identity=idb[:]),
                     reads=[xbB, idbB], writes=[pTB])
            S.op("dve", lambda E, jj=jj: E.tensor_copy(out=xT[:, :, jj * 128:(jj + 1) * 128], in_=pT[:]), reads=[pTB], writes=[xTB])

        if even:
            for c in range(4):
                pu, puB = pacc.get()
                mm_chain(S, pu[:, 0:NT], puB, [(win[:, k, c * 128:(c + 1) * 128], xT[:, k, :]) for k in range(8)], reads=[winB, xTB])
                S.op("act", lambda E, c=c, pu=pu: E.activation(out=ug[:, c, :], in_=pu[:, 0:NT], func=AF.Gelu), reads=[puB], writes=[ugB[c]])
            for jj in range(GT):
                pv, pvB = pacc.get()
                mm_chain(S, pv[:], pvB, [(xT[:, k, jj * 128:(jj + 1) * 128], win[:, k, 512:1024]) for k in range(8)], reads=[winB, xTB])
                vg, vgB = sm512.get()
                S.op("act", lambda E, vg=vg, pv=pv: E.activation(out=vg[:], in_=pv[:], func=AF.Gelu), reads=[pvB], writes=[vgB])
                stats, stB = lnst["stats"].get()
                mv, mvB = lnst["mv"].get()
                rstd, rsB = lnst["rstd"].get()
                S.op("dve", lambda E, vg=vg, stats=stats: E.bn_stats(out=stats[:, 0, :], in_=vg[:]), reads=[vgB], writes=[stB])
                S.op("dve", lambda E, stats=stats, mv=mv: E.bn_aggr(out=mv[:], in_=stats[:, 0, :]), reads=[stB], writes=[mvB])
                S.op("act", lambda E, rstd=rstd, mv=mv: E.activation(out=rstd[:, 0:1], in_=mv[:, 1:2], func=AF.Sqrt,
                                                                   bias=lnst["eps"][0][:, 0:1], scale=1.0),
                     reads=[mvB, lnst["eps"][1]], writes=[rsB])
                S.op("dve", lambda E, rstd=rstd: E.reciprocal(out=rstd[:, 1:2], in_=rstd[:, 0:1]), reads=[rsB], writes=[rsB])
                S.op("dve", lambda E, vg=vg, mv=mv, rstd=rstd: E.tensor_scalar(
                    out=vg[:], in0=vg[:], scalar1=mv[:, 0:1], scalar2=rstd[:, 1:2], op0=ALU.subtract, op1=ALU.mult),
                    reads=[vgB, mvB, rsB], writes=[vgB])
                vnb, vnbB = vnb_r.get()
                S.op("dve", lambda E, vnb=vnb, vg=vg: E.tensor_tensor(out=vnb[:], in0=vg[:], in1=ang[:], op=ALU.mult),
                     reads=[vgB, angB], writes=[vnbB])
                for c in range(4):
                    psv, psvB = pacc.get()
                    S.op_nosig("pe", lambda E, c=c, psv=psv, vnb=vnb: E.matmul(psv[:, 0:128], lhsT=vnb[:, c * 128:(c + 1) * 128],
                                                                              rhs=wsT[:, 2 * c, :], start=True, stop=True),
                               reads=[vnbB, wsTB], writes=[psvB])
                    S.op("pe", lambda E, c=c, psv=psv, vnb=vnb: E.matmul(psv[:, 128:256], lhsT=vnb[:, c * 128:(c + 1) * 128],
                                                                        rhs=wsT[:, 2 * c + 1, :], start=True, stop=True),
                         reads=[vnbB, wsTB], writes=[psvB])
                    sv, svB = sv128.get()
                    S.op("dve", lambda E, c=c, psv=psv, sv=sv: E.tensor_tensor(out=sv[0:64, :], in0=psv[0:64, 0:128], in1=bsT[0:64, c, :], op=ALU.add),
                         reads=[psvB, bsTB], writes=[svB])
                    S.op("dve", lambda E, c=c, psv=psv, sv=sv: E.tensor_tensor(out=sv[64:128, :], in0=psv[64:128, 128:256], in1=bsT[64:128, c, :], op=ALU.add),
                         reads=[psvB, bsTB], writes=[svB])
                    S.op("dve", lambda E, c=c, sv=sv, jj=jj: E.tensor_tensor(out=yT[:, c, jj * 128:(jj + 1) * 128], in0=sv[:],
                                                                            in1=ug[:, c, jj * 128:(jj + 1) * 128], op=ALU.mult),
                         reads=[svB, ugB[c]], writes=[yTB])
            for c in range(4):
                pxi, pxiB = pacc.get()
                mm_chain(S, pxi[:, 0:NT], pxiB, [(win[:, k, 2048 + c * 128:2048 + (c + 1) * 128], xT[:, k, :]) for k in range(8)], reads=[winB, xTB])
                xi, xiB = fm.get()
                S.op("act", lambda E, xi=xi, pxi=pxi: E.copy(out=xi[:], in_=pxi[:, 0:NT]), reads=[pxiB], writes=[xiB])
                pgc, pgcB = pacc.get()
                mm_chain(S, pgc[:, 0:NT], pgcB, [(win[:, k, 1536 + c * 128:1536 + (c + 1) * 128], xT[:, k, :]) for k in range(8)], reads=[winB, xTB])
                S.op("dve", lambda E, c=c, pgc=pgc, xi=xi: E.tensor_tensor(out=zc[:, c, 2:2 + NT], in0=pgc[:, 0:NT], in1=xi[:], op=ALU.mult),
                     reads=[pgcB, xiB], writes=[zcB[c]])
                o1, o1B = fm.get()
                S.op("dve", lambda E, c=c, o1=o1: E.tensor_scalar(out=o1[:], in0=zc[:, c, 0:NT], scalar1=cw[:, c, 0:1], scalar2=None, op0=ALU.mult),
                     reads=[zcB[c], cwB], writes=[o1B])
                S.op("dve", lambda E, c=c, o1=o1: E.scalar_tensor_tensor(out=o1[:], in0=zc[:, c, 1:NT + 1], scalar=cw[:, c, 1:2], in1=o1[:],
                                                                        op0=ALU.mult, op1=ALU.add), reads=[zcB[c], cwB, o1B], writes=[o1B])
                S.op("dve", lambda E, c=c, o1=o1: E.scalar_tensor_tensor(out=o1[:], in0=zc[:, c, 2:NT + 2], scalar=cw[:, c, 2:3], in1=o1[:],
                                                                        op0=ALU.mult, op1=ALU.add), reads=[zcB[c], cwB, o1B], writes=[o1B])
                cr, crB = cr_r.get()
                S.op("act", lambda E, c=c, cr=cr: E.copy(out=cr[:, 0:2], in_=zc[:, c, NT:NT + 2]), reads=[zcB[c]], writes=[crB])
                S.op("act", lambda E, c=c, cr=cr: E.copy(out=zc[:, c, 0:2], in_=cr[:, 0:2]), reads=[crB, zcB[c]], writes=[zcB[c]])
                pgb, pgbB = pacc.get()
                mm_chain(S, pgb[:, 0:NT], pgbB, [(win[:, k, 1024 + c * 128:1024 + (c + 1) * 128], xT[:, k, :]) for k in range(8)], reads=[winB, xTB])
                S.op("dve", lambda E, c=c, pgb=pgb, o1=o1: E.tensor_tensor(out=yT[:, 4 + c, :], in0=pgb[:, 0:NT], in1=o1[:], op=ALU.mult),
                     reads=[pgbB, o1B], writes=[yTB])
        else:
            ic, icB = invc_r.get()
            S.dma("sp", lambda E, ic=ic, g0=g0: E.dma_start(
                out=ic[:], in_=invc_d[:, g0 * 128:g0 * 128 + NT].partition_broadcast(128)), "invc", writes=[icB])
            for c in range(8):
                pz, pzB = pacc.get()
                mm_chain(S, pz[:, 0:NT], pzB, [(cin[:, k, c * 128:(c + 1) * 128], xT[:, k, :]) for k in range(8)], reads=[cinB, xTB])
                S.op("act", lambda E, c=c, pz=pz: E.copy(out=ze[:, c, 15:15 + NT], in_=pz[:, 0:NT]), reads=[pzB], writes=[zeB[c]])
                gi = c // 2
                src, srcB = ze[:, c, :], zeB[c]
                lo = 0
                for stp in range(gi + 1):
                    sh = 1 << stp
                    nlo = lo + sh
                    dstt, dstB = sAB.get()
                    S.op("dve", lambda E, src=src, dstt=dstt, nlo=nlo, sh=sh: E.tensor_tensor(
                        out=dstt[:, nlo:NT + 15], in0=src[:, nlo:NT + 15], in1=src[:, nlo - sh:NT + 15 - sh], op=ALU.add),
                        reads=[srcB], writes=[dstB])
                    src, srcB, lo = dstt, dstB, nlo
                pl_, plB_ = fm.get()
                S.op("dve", lambda E, src=src, pl_=pl_, gi=gi, ic=ic: E.tensor_tensor(out=pl_[:], in0=src[:, 15:15 + NT], in1=ic[:, gi, :], op=ALU.mult),
                     reads=[srcB, icB], writes=[plB_])
                S.op("dve", lambda E, c=c, pl_=pl_: E.tensor_tensor(out=pooled[:, c, :], in0=pl_[:], in1=ze[:, c, 15:15 + NT], op=ALU.subtract),
                     reads=[plB_, zeB[c]], writes=[pooledB[c]])
                cr, crB = cr_r.get()
                S.op("act", lambda E, c=c, cr=cr: E.copy(out=cr[:, 0:15], in_=ze[:, c, NT:NT + 15]), reads=[zeB[c]], writes=[crB])
                S.op("act", lambda E, c=c, cr=cr: E.copy(out=ze[:, c, 0:15], in_=cr[:, 0:15]), reads=[crB, zeB[c], pooledB[c]], writes=[zeB[c]])
            for c2 in range(8):
                gi = c2 // 2
                pmx, pmxB = pacc.get()
                mm_chain(S, pmx[:, 0:NT], pmxB, [(wg[:, gi, cc, (c2 % 2) * 128:(c2 % 2 + 1) * 128], pooled[:, 2 * gi + cc, :]) for cc in range(2)],
                         reads=[wgB, pooledB[2 * gi], pooledB[2 * gi + 1]])
                S.op("act", lambda E, c2=c2, pmx=pmx: E.activation(out=yT[:, c2, :], in_=pmx[:, 0:NT], func=AF.Copy, scale=csc[:, c2:c2 + 1]),
                     reads=[pmxB, cscB], writes=[yTB])

        for jj in range(GT):
            post_mix_tile(g0 + jj, jj, xts[jj][0], xts[jj][1], wout, woutB)

    S.drain()
    S.dma("sp", lambda E: E.dma_start(out=dest_out, in_=dest_all[:]), "fin", reads=[destB], writes=[Buf("o1")])
    S.dma("sp", lambda E: E.dma_start(out=gate_out, in_=gate_all[:]), "fin", reads=[gateB], writes=[Buf("o2")])


A_IN_COMMON = [("tmask", [128, NTL], F32), ("lnm_g", [D], F32), ("lnm_b", [D], F32), ("router_w", [D, NE], F32),
               ("router_b", [NE], F32), ("w_out", [D, D], F32)]
A_IN_EVEN = [("w_in", [D, 2560], F32), ("a_norm_g", [512], F32), ("a_w_s", [8, 128, 128], F32), ("a_b_s", [8, 128], F32),
             ("b_conv_w", [3, 512], F32)]
A_IN_ODD = [("w_in", [D, D], F32), ("c_w_grp", [4, 256, 256], F32), ("c_scale", [D], F32), ("invc", [4, T], F32)]
A_IN_PREV = [("x1p", [T, D], F32), ("yr", [XS_ROWS, D], F32), ("destp", [128, NTL * TOPK], U32),
             ("gatep", [128, NTL * TOPK], F32), ("lnf_g_prev", [D], F32), ("lnf_b_prev", [D], F32)]
A_OUT = [("xs", [XS_ROWS, D], BF16), ("x1", [T, D], F32), ("dest", [128, NTL * TOPK], U32), ("gate", [128, NTL * TOPK], F32)]


def load_tmask(K, tmask_d):
    tm = K.sb("tmask_sb", [128, NTL], F32)
    tmB = Buf("tmask")
    K.S.dma("sp", lambda E: E.dma_start(out=tm[:], in_=tmask_d), K.ckey(), writes=[tmB])
    return tm, tmB


def build_A(layer, first):
    K = Ctx("A%d" % layer)
    cs = load_consts(K)
    lnst = make_ln_state(K)
    io = {}
    specs = A_IN_COMMON + (A_IN_EVEN if layer % 2 == 0 else A_IN_ODD) + ([("x", [T, D], F32)] if first else A_IN_PREV)
    for (nm, shp, dt) in specs:
        io[nm] = K.dram_in(nm, shp, dt)
    for (nm, shp, dt) in A_OUT:
        io[nm] = K.dram_out(nm, shp, dt)
    if not first:
        io["x_mid"] = K.dram_tmp("x_mid", [T, D], F32)
    tm, tmB = load_tmask(K, io["tmask"])
    emit_A(K, layer, first, io, cs, lnst, tm, tmB)
    return K.finish()


def expert_phase(K, cs, xr, ys, wup_d, bup_d, wdn_d, bdn_d, xrB, ysB, n_exp, tile_fn, gw):
    S = K.S
    idb, idbB = cs["ident_bf"]
    NW = gw * 128
    wu_r = Ring(K.sb, "wu", [128, 8, 2048], BF16, 2)
    wd_r = Ring(K.sb, "wd", [128, 8, D], BF16, 2)
    bup_r = Ring(K.sb, "bup", [128, 16], F32, 2)
    bdn_r = Ring(K.sb, "bdn", [128, D], F32, 2)
    xg_r = Ring(K.sb, "xg", [128, D], BF16, 6)
    xgT_r = Ring(K.sb, "xgT", [128, 8, NW], BF16, 2)
    actT_r = Ring(K.sb, "actT", [128, 8, NW], BF16, 2)
    g_r = Ring(K.sb, "sw_g", [128, NW], F32, 3)
    sg_r = Ring(K.sb, "sw_s", [128, NW], F32, 3)
    l_r = Ring(K.sb, "sw_l", [128, NW], F32, 3)
    ysb_r = Ring(K.sb, "ysb", [128, D], F32, 2)
    pT_r = Ring(K.ps, "pTe", [128, 8, 128], BF16, 2)
    pacc = Ring(K.ps, "pacce", [128, 512], F32, 6)

    def load_wu(le):
        wu, wuB = wu_r.get()
        for hh in range(2):
            S.dma("pool", lambda E, hh=hh, wu=wu: E.dma_start(
                out=wu[:, hh * 4:(hh + 1) * 4, :], in_=wup_d[le].rearrange("(k p) n -> p k n", p=128)[:, hh * 4:(hh + 1) * 4, :]),
                "wu%d" % hh, writes=[wuB])
        return (wu, wuB)

    def load_rest(le):
        wd, wdB = wd_r.get()
        bup, bupB = bup_r.get()
        bdn, bdnB = bdn_r.get()
        S.dma("pool", lambda E, wd=wd: E.dma_start(out=wd[:], in_=wdn_d[le].rearrange("(k p) n -> p k n", p=128)), "wd", writes=[wdB])
        S.dma("sp", lambda E, bup=bup: E.dma_start(out=bup[:], in_=bup_d[le].rearrange("(f p) -> p f", p=128),
                                                  allow_slow_non_contiguous=True), "bup", writes=[bupB])
        S.dma("sp", lambda E, bdn=bdn: E.dma_start(out=bdn[:], in_=bdn_d[le].partition_broadcast(128)), "bdn", writes=[bdnB])
        return (wd, wdB, bup, bupB, bdn, bdnB)

    groups = []
    for le in range(n_exp):
        tiles = tile_fn(le)
        for q0 in range(0, len(tiles), gw):
            groups.append((le, tiles[q0:q0 + gw]))

    def stage_T(gi):
        le, grp = groups[gi]
        xgT, xgTB = xgT_r.get()
        for q, r0 in enumerate(grp):
            xg, xgB = xg_r.get()
            S.dma("sp", lambda E, xg=xg, r0=r0: E.dma_start(out=xg[:], in_=xr[r0:r0 + 128, :]), "xg%d" % q, reads=[xrB], writes=[xgB])
            pT, pTB = pT_r.get()
            for k in range(8):
                fn = lambda E, k=k, xg=xg, pT=pT: E.transpose(out=pT[:, k, :], in_=xg[:, k * 128:(k + 1) * 128], identity=idb[:])
                if k < 7:
                    S.op_nosig("pe", fn, reads=[xgB, idbB] if k == 0 else (), writes=[pTB] if k == 0 else ())
                else:
                    S.op("pe", fn, reads=[xgB, idbB], writes=[pTB])
            S.op("act", lambda E, q=q, pT=pT, xgT=xgT: E.copy(out=xgT[:, :, q * 128:(q + 1) * 128], in_=pT[:]), reads=[pTB], writes=[xgTB])
        return xgT, xgTB

    wucache = {}
    wrcache = {}

    def get_wu(le):
        if le not in wucache:
            wucache[le] = load_wu(le)
        return wucache[le]

    def get_rest(le):
        if le not in wrcache:
            wrcache[le] = load_rest(le)
        return wrcache[le]

    def weights(le):
        wu, wuB = get_wu(le)
        wd, wdB, bup, bupB, bdn, bdnB = get_rest(le)
        return (wu, wuB, wd, wdB, bup, bupB, bdn, bdnB)

    weights(0)
    if n_exp > 1:
        get_wu(1)
    cur = stage_T(0)
    for gi, (le, grp) in enumerate(groups):
        wu, wuB, wd, wdB, bup, bupB, bdn, bdnB = weights(le)
        if le + 1 < n_exp and (gi == 0 or groups[gi - 1][0] != le):
            get_rest(le + 1)
        xgT, xgTB = cur
        nw = len(grp) * 128
        actT, actTB = actT_r.get()
        pend = None
        for fc in range(9):
            if fc < 8:
                phg, phgB = pacc.get()
                mm_chain(S, phg[:, 0:nw], phgB, [(wu[:, k, fc * 128:(fc + 1) * 128], xgT[:, k, 0:nw]) for k in range(8)], reads=[wuB, xgTB])
                phl, phlB = pacc.get()
                mm_chain(S, phl[:, 0:nw], phlB, [(wu[:, k, (fc + 8) * 128:(fc + 9) * 128], xgT[:, k, 0:nw]) for k in range(8)], reads=[wuB, xgTB])
                g, gB = g_r.get()
                S.op("dve", lambda E, g=g, phg=phg, fc=fc, bup=bup: E.tensor_scalar(out=g[:, 0:nw], in0=phg[:, 0:nw], scalar1=bup[:, fc:fc + 1], scalar2=7.0,
                                                                              op0=ALU.add, op1=ALU.min), reads=[phgB, bupB], writes=[gB])
                sg, sgB = sg_r.get()
                S.op("act", lambda E, g=g, sg=sg: E.activation(out=sg[:, 0:nw], in_=g[:, 0:nw], func=AF.Sigmoid, scale=1.702), reads=[gB], writes=[sgB])
                l1, l1B = l_r.get()
                S.op("dve", lambda E, l1=l1, phl=phl, fc=fc, bup=bup: E.tensor_scalar(out=l1[:, 0:nw], in0=phl[:, 0:nw], scalar1=bup[:, fc + 8:fc + 9], scalar2=7.0,
                                                                                op0=ALU.add, op1=ALU.min), reads=[phlB, bupB], writes=[l1B])
                S.op("dve", lambda E, l1=l1: E.tensor_scalar(out=l1[:, 0:nw], in0=l1[:, 0:nw], scalar1=-7.0, scalar2=1.0, op0=ALU.max, op1=ALU.add),
                     reads=[l1B], writes=[l1B])
                new_pend = (fc, g, gB, sg, sgB, l1, l1B)
            else:
                new_pend = None
            if pend is not None:
                pfc, pg, pgB, psg, psgB, pl1, pl1B = pend
                S.op("dve", lambda E, pg=pg, psg=psg: E.tensor_tensor(out=psg[:, 0:nw], in0=pg[:, 0:nw], in1=psg[:, 0:nw], op=ALU.mult),
                     reads=[pgB, psgB], writes=[psgB])
                S.op("dve", lambda E, pl1=pl1, psg=psg, pfc=pfc, actT=actT: E.tensor_tensor(out=actT[:, pfc, 0:nw], in0=pl1[:, 0:nw], in1=psg[:, 0:nw], op=ALU.mult),
                     reads=[pl1B, psgB], writes=[actTB])
            pend = new_pend
        last_of_expert = (gi + 1 == len(groups)) or (groups[gi + 1][0] != le)
        if last_of_expert and le + 2 < n_exp:
            get_wu(le + 2)
        if gi + 1 < len(groups):
            nle = groups[gi + 1][0]
            weights(nle)
            cur = stage_T(gi + 1)
        for q, r0 in enumerate(grp):
            ysb, ysbB = ysb_r.get()
            for n in range(2):
                py, pyB = pacc.get()
                mm_chain(S, py[:], pyB, [(actT[:, fc, q * 128:(q + 1) * 128], wd[:, fc, n * 512:(n + 1) * 512]) for fc in range(8)],
                         reads=[actTB, wdB])
                S.op("dve", lambda E, n=n, py=py, ysb=ysb, bdn=bdn: E.tensor_tensor(
                    out=ysb[:, n * 512:(n + 1) * 512], in0=py[:], in1=bdn[:, n * 512:(n + 1) * 512], op=ALU.add),
                    reads=[pyB, bdnB], writes=[ysbB])
            S.dma("pool", lambda E, ysb=ysb, r0=r0: E.dma_start(out=ys[r0:r0 + 128, :], in_=ysb[:]), "yso%d" % (q % 2), reads=[ysbB], writes=[ysB])


def build_B():
    K = Ctx("B")
    cs = load_consts(K, need_router=False)
    xr = K.dram_in("xr", [NSLOT, D], BF16)
    wup_d = K.dram_in("w_up", [EPC, D, 2048], F32)
    bup_d = K.dram_in("b_up", [EPC, 2048], F32)
    wdn_d = K.dram_in("w_down", [EPC, D, D], F32)
    bdn_d = K.dram_in("b_down", [EPC, D], F32)
    ys = K.dram_out("ys", [NSLOT, D], F32)
    expert_phase(K, cs, xr, ys, wup_d, bup_d, wdn_d, bdn_d, Buf("xr"), Buf("ys"), EPC,
                 lambda le: [(c * EPC + le) * C + s_ * 128 for c in range(NCORES) for s_ in range(C // 128)], 4)
    return K.finish()


def build_C():
    K = Ctx("C")
    S = K.S
    cs = load_consts(K, need_router=False)
    lnst = make_ln_state(K)
    big = Ring(K.sb, "big", [128, D], F32, 8)
    tmask_d = K.dram_in("tmask", [128, NTL], F32)
    tm = K.sb("tmask_sb", [128, NTL], F32)
    tmB = Buf("tmask")
    S.dma("sp", lambda E: E.dma_start(out=tm[:], in_=tmask_d), K.ckey(), writes=[tmB])
    x1p = K.dram_in("x1p", [T, D], F32)
    yr = K.dram_in("yr", [XS_ROWS, D], F32)
    destp = K.dram_in("destp", [128, NTL * TOPK], U32)
    gatep = K.dram_in("gatep", [128, NTL * TOPK], F32)
    lnfg = K.dram_in("lnf_g_prev", [D], F32)
    lnfb = K.dram_in("lnf_b_prev", [D], F32)
    out = K.dram_out("out", [TOK, D], F32)
    nh = HALO // 128
    combine_phase(K, cs, lnst, x1p, yr, destp, gatep, lnfg, lnfb, (tm, tmB),
                  lambda j: (out[(j - nh) * 128:(j - nh + 1) * 128, :] if j >= nh else None), big)
    return K.finish()


def build_fused(depth=DEPTH):
    K = Ctx("F")
    S = K.S
    cs = load_consts(K)
    lnst = make_ln_state(K)
    tm, tmB = load_tmask(K, K.dram_in("tmask", [128, NTL], F32))
    x = K.dram_in("x", [T, D], F32)
    invc = K.dram_in("invc", [4, T], F32)
    out = K.dram_out("out", [TOK, D], F32)
    xs = K.dram_tmp("xs_i", [XS_ROWS, D], BF16)
    ys = K.dram_tmp("ys_i", [XS_ROWS, D], F32)
    x1d = [K.dram_tmp("x1_i%d" % i, [T, D], F32) for i in range(2)]
    xmid = K.dram_tmp("xmid_i", [T, D], F32)
    dsd = [K.dram_tmp("dest_i%d" % i, [128, NTL * TOPK], U32) for i in range(2)]
    gtd = [K.dram_tmp("gate_i%d" % i, [128, NTL * TOPK], F32) for i in range(2)]
    W = []
    for l in range(depth):
        w = {}
        specs = [s_ for s_ in A_IN_COMMON if s_[0] != "tmask"] + (A_IN_EVEN if l % 2 == 0 else [s_ for s_ in A_IN_ODD if s_[0] != "invc"])
        for (nm, shp, dt) in specs:
            w[nm] = K.dram_in("%s_%d" % (nm, l), shp, dt)
        w["lnf_g"] = K.dram_in("lnf_g_%d" % l, [D], F32)
        w["lnf_b"] = K.dram_in("lnf_b_%d" % l, [D], F32)
        w["w_up"] = K.dram_in("w_up_%d" % l, [NE, D, 2048], F32)
        w["b_up"] = K.dram_in("b_up_%d" % l, [NE, 2048], F32)
        w["w_down"] = K.dram_in("w_down_%d" % l, [NE, D, D], F32)
        w["b_down"] = K.dram_in("b_down_%d" % l, [NE, D], F32)
        W.append(w)

    K.begin_phase()
    zt = K.sb("zeros", [128, 4096], F32)
    zB = Buf("zeros")
    S.op("pool", lambda E: E.memset(zt[:], 0.0), writes=[zB])
    ztb = zt[:].bitcast(BF16)
    scrB = Buf("scratch")
    for i, r0 in enumerate(range(0, XS_ROWS - 128, 1024)):
        nr = min(1024, XS_ROWS - r0)
        S.dma("sp", lambda E, r0=r0, nr=nr: E.dma_start(out=xs[r0:r0 + nr, :].rearrange("(p a) n -> p (a n)", p=128),
                                                       in_=ztb[:, 0:(nr // 128) * D]), "z%d" % (i % 4), reads=[zB], writes=[scrB])
    S.dma("sp", lambda E: E.dma_start(out=xs[NSLOT:XS_ROWS, :], in_=ztb[:, 0:D]), "z0", reads=[zB], writes=[scrB])
    S.dma("sp", lambda E: E.dma_start(out=ys[NSLOT:XS_ROWS, :], in_=zt[:, 0:D]), "z1", reads=[zB], writes=[scrB])
    K.end_phase()

    for l in range(depth):
        io = dict(W[l])
        io["invc"] = invc
        io["xs"] = xs
        io["x1"] = x1d[l % 2]
        io["dest"] = dsd[l % 2]
        io["gate"] = gtd[l % 2]
        if l == 0:
            io["x"] = x
        else:
            K.begin_phase()
            cbig = Ring(K.sb, "cbig", [128, D], F32, 28)
            combine_phase(K, cs, lnst, x1d[(l - 1) % 2], ys, dsd[(l - 1) % 2], gtd[(l - 1) % 2],
                          W[l - 1]["lnf_g"], W[l - 1]["lnf_b"], (tm, tmB),
                          lambda j: xmid[j * 128:(j + 1) * 128, :], cbig)
            K.end_phase()
            io["x"] = xmid
        K.begin_phase()
        emit_A(K, l, True, io, cs, lnst, tm, tmB)
        K.end_phase()
        K.begin_phase()
        expert_phase(K, cs, xs, ys, W[l]["w_up"], W[l]["b_up"], W[l]["w_down"], W[l]["b_down"], Buf("xr"), Buf("ys"), NE,
                     lambda le: [le * C + s_ * 128 for s_ in range(C // 128)], C // 128)
        K.end_phase()
    K.begin_phase()
    big = Ring(K.sb, "bigc", [128, D], F32, 28)
    nh = HALO // 128
    lp = depth - 1
    combine_phase(K, cs, lnst, x1d[lp % 2], ys, dsd[lp % 2], gtd[lp % 2], W[lp]["lnf_g"], W[lp]["lnf_b"], (tm, tmB),
                  lambda j: (out[(j - nh) * 128:(j - nh + 1) * 128, :] if j >= nh else None), big)
    K.end_phase()
    return K.finish()


def _consts():
    return {
        "ident_bf": np.eye(128).astype(ml_dtypes.bfloat16),
        "ident_f": np.eye(128).astype(np.float32),
        "triu": np.triu(np.ones((128, 128), np.float32), 1).astype(ml_dtypes.bfloat16),
        "ones_bf": np.ones((128, 128), np.float32).astype(ml_dtypes.bfloat16),
        "tril": np.tril(np.ones((128, 128), np.float32)),
        "iota": np.tile(np.arange(NE, dtype=np.float32), (128, 1)),
        "dump_p": (DUMP + np.arange(128, dtype=np.float32)).reshape(128, 1),
    }


def _core_consts(c):
    start = c * TOK - HALO
    gpos = start + np.arange(T)
    tmask = np.ascontiguousarray((gpos >= 0).astype(np.float32).reshape(NTL, 128).T)
    invc = np.stack([1.0 / np.minimum(np.maximum(gpos, 0) + 1, w) for w in (2, 4, 8, 16)]).astype(np.float32)
    return tmask, invc


_PROGS = {}


def _prog(key, fn):
    if key not in _PROGS:
        _PROGS[key] = fn()
    return _PROGS[key]


def kernel_unfused(x, ab_w_in, a_norm_g, a_w_s, a_b_s, b_conv_w, ab_w_out, c_w_in, c_w_grp, c_scale, c_w_out,
           ln_mix_g, ln_mix_b, router_w, router_b, moe_w_up, moe_b_up, moe_w_down, moe_b_down, ln_ffn_g, ln_ffn_b):
    f32 = lambda a: np.ascontiguousarray(np.asarray(a, dtype=np.float32))
    x = f32(x)[0]
    cst = _consts()
    cc = [_core_consts(c) for c in range(NCORES)]
    cores = list(range(NCORES))
    rout_keys = ("ident_bf", "ident_f", "triu", "ones_bf", "tril", "iota", "dump_p")
    state = None
    for layer in range(DEPTH):
        i = layer // 2
        first = (layer == 0)
        nc = _prog(("A", layer % 2, first), lambda: build_A(layer, first))
        maps = []
        for c in cores:
            m = {k: cst[k] for k in rout_keys}
            m["tmask"] = cc[c][0]
            m["lnm_g"] = f32(ln_mix_g[layer])
            m["lnm_b"] = f32(ln_mix_b[layer])
            m["router_w"] = f32(router_w[layer])
            m["router_b"] = f32(router_b[layer])
            if layer % 2 == 0:
                m.update({"w_in": f32(ab_w_in[i]), "w_out": f32(ab_w_out[i]), "a_norm_g": f32(a_norm_g[i]),
                          "a_w_s": f32(a_w_s[i]), "a_b_s": f32(a_b_s[i]), "b_conv_w": f32(b_conv_w[i])})
            else:
                m.update({"w_in": f32(c_w_in[i]), "w_out": f32(c_w_out[i]), "c_w_grp": f32(c_w_grp[i]),
                          "c_scale": f32(c_scale[i]), "invc": cc[c][1]})
            if first:
                start = c * TOK - HALO
                lo = max(start, 0)
                xl = np.zeros((T, D), np.float32)
                xl[lo - start:] = x[lo:start + T]
                m["x"] = xl
            else:
                m.update({"x1p": state[c]["x1"], "yr": state[c]["yr"], "destp": state[c]["dest"], "gatep": state[c]["gate"],
                          "lnf_g_prev": f32(ln_ffn_g[layer - 1]), "lnf_b_prev": f32(ln_ffn_b[layer - 1])})
            maps.append(m)
        resA = run_bass_kernel_spmd(nc, maps, core_ids=cores).results
        ncb = _prog(("B",), build_B)
        mapsB = []
        for d in cores:
            xr = np.concatenate([resA[c]["xs"][d * EPC * C:(d + 1) * EPC * C] for c in cores], axis=0)
            mapsB.append({"ident_bf": cst["ident_bf"], "ident_f": cst["ident_f"], "xr": xr,
                          "w_up": f32(moe_w_up[layer, d * EPC:(d + 1) * EPC]), "b_up": f32(moe_b_up[layer, d * EPC:(d + 1) * EPC]),
                          "w_down": f32(moe_w_down[layer, d * EPC:(d + 1) * EPC]), "b_down": f32(moe_b_down[layer, d * EPC:(d + 1) * EPC])})
        resB = run_bass_kernel_spmd(ncb, mapsB, core_ids=cores).results
        state = []
        for c in cores:
            yr = np.zeros((XS_ROWS, D), np.float32)
            for d in cores:
                yr[d * EPC * C:(d + 1) * EPC * C] = resB[d]["ys"][c * EPC * C:(c + 1) * EPC * C]
            state.append({"x1": resA[c]["x1"], "dest": resA[c]["dest"], "gate": resA[c]["gate"], "yr": yr})
        del resA, resB
    ncc = _prog(("C",), build_C)
    mapsC = []
    for c in cores:
        mapsC.append({"ident_bf": cst["ident_bf"], "ident_f": cst["ident_f"], "tmask": cc[c][0],
                      "x1p": state[c]["x1"], "yr": state[c]["yr"], "destp": state[c]["dest"], "gatep": state[c]["gate"],
                      "lnf_g_prev": f32(ln_ffn_g[DEPTH - 1]), "lnf_b_prev": f32(ln_ffn_b[DEPTH - 1])})
    resC = run_bass_kernel_spmd(ncc, mapsC, core_ids=cores).results
    out = np.concatenate([resC[c]["out"] for c in cores], axis=0)
    return out.reshape(1, SEQ, D).astype(np.float32)


def kernel(x, ab_w_in, a_norm_g, a_w_s, a_b_s, b_conv_w, ab_w_out, c_w_in, c_w_grp, c_scale, c_w_out,
           ln_mix_g, ln_mix_b, router_w, router_b, moe_w_up, moe_b_up, moe_w_down, moe_b_down, ln_ffn_g, ln_ffn_b):
    f32 = lambda a: np.ascontiguousarray(np.asarray(a, dtype=np.float32))
    x = f32(x)[0]
    cst = _consts()
    shared = {}
    for l in range(DEPTH):
        i = l // 2
        shared.update({"lnm_g_%d" % l: f32(ln_mix_g[l]), "lnm_b_%d" % l: f32(ln_mix_b[l]), "router_w_%d" % l: f32(router_w[l]),
                       "router_b_%d" % l: f32(router_b[l]), "lnf_g_%d" % l: f32(ln_ffn_g[l]), "lnf_b_%d" % l: f32(ln_ffn_b[l]),
                       "w_up_%d" % l: f32(moe_w_up[l]), "b_up_%d" % l: f32(moe_b_up[l]),
                       "w_down_%d" % l: f32(moe_w_down[l]), "b_down_%d" % l: f32(moe_b_down[l])})
        if l % 2 == 0:
            shared.update({"w_in_%d" % l: f32(ab_w_in[i]), "w_out_%d" % l: f32(ab_w_out[i]), "a_norm_g_%d" % l: f32(a_norm_g[i]),
                           "a_w_s_%d" % l: f32(a_w_s[i]), "a_b_s_%d" % l: f32(a_b_s[i]), "b_conv_w_%d" % l: f32(b_conv_w[i])})
        else:
            shared.update({"w_in_%d" % l: f32(c_w_in[i]), "w_out_%d" % l: f32(c_w_out[i]), "c_w_grp_%d" % l: f32(c_w_grp[i]),
                           "c_scale_%d" % l: f32(c_scale[i])})
    maps = []
    for c in range(NCORES):
        tmask, invc = _core_consts(c)
        start = c * TOK - HALO
        lo = max(start, 0)
        xl = np.zeros((T, D), np.float32)
        xl[lo - start:] = x[lo:start + T]
        m = dict(cst)
        m.update(shared)
        m.update({"x": xl, "tmask": tmask, "invc": invc})
        maps.append(m)
    nc = _prog(("F",), build_fused)
    res = run_bass_kernel_spmd(nc, maps, core_ids=list(range(NCORES))).results
    out = np.concatenate([res[c]["out"] for c in range(NCORES)], axis=0)
    return out.reshape(1, SEQ, D).astype(np.float32)
```

```python
import contextlib
import numpy as np
import ml_dtypes
import concourse.bass as bass
import concourse.mybir as mybir
from concourse.bass_utils import run_bass_kernel_spmd

F32 = mybir.dt.float32
BF16 = mybir.dt.bfloat16
U32 = mybir.dt.uint32
ALU = mybir.AluOpType
AF = mybir.ActivationFunctionType

NCORES = 8
D = 1024
SEQ = 16384
TOK = SEQ // NCORES
HALO = 256
T = TOK + HALO
NTL = T // 128
NE = 32
TOPK = 4
EPC = NE // NCORES
C = 384
NSLOT = NE * C
DUMP = NSLOT
XS_ROWS = NSLOT + 128
ALPHA = float(8 ** 0.25)
EPS = 1e-5
GT = 3
NT = GT * 128
DEPTH = 4

ENGS = ("pe", "dve", "act", "pool", "sp")


class Buf:
    __slots__ = ("name", "w", "rs")

    def __init__(self, name):
        self.name = name
        self.w = None
        self.rs = []


class Sched:
    def __init__(self, nc, stack):
        self.nc = nc
        self.stack = stack
        self.streams = {e: [] for e in ENGS}
        self.esem = {e: stack.enter_context(nc.semaphore("s_" + e)) for e in ENGS}
        self.ecnt = {e: 0 for e in ENGS}
        self.waited = {e: {} for e in ENGS}
        self.dsem = {}
        self.bg = None
        self._in_bg = False

    def _tick(self):
        if self.bg is not None and not self._in_bg:
            self._in_bg = True
            try:
                next(self.bg)
            except StopIteration:
                self.bg = None
            self._in_bg = False

    def drain(self):
        while self.bg is not None:
            self._tick()

    def _deps(self, eng, reads, writes):
        deps = []
        for b in reads:
            if b.w is not None:
                deps.append(b.w)
        for b in writes:
            if b.w is not None and b.w[0] != eng:
                deps.append(b.w)
            for r in b.rs:
                if r[0] != eng:
                    deps.append(r)
        need = {}
        for (e, sem, val) in deps:
            k = id(sem)
            if k not in need or need[k][1] < val:
                need[k] = (sem, val)
        out = []
        w = self.waited[eng]
        for k, (sem, val) in need.items():
            if w.get(k, 0) < val:
                w[k] = val
                out.append((sem, val))
        return out

    def _commit(self, tok, reads, writes):
        for b in writes:
            b.w = tok
            b.rs = []
        for b in reads:
            b.rs.append(tok)
            if len(b.rs) > 16:
                latest = {}
                for r in b.rs:
                    k = id(r[1])
                    if k not in latest or latest[k][2] < r[2]:
                        latest[k] = r
                b.rs = list(latest.values())

    def op(self, eng, fn, reads=(), writes=()):
        waits = self._deps(eng, reads, writes)
        self.ecnt[eng] += 1
        sem = self.esem[eng]
        tok = (eng, sem, self.ecnt[eng])

        def run(E, waits=waits, fn=fn, sem=sem):
            for (s, v) in waits:
                E.wait_ge(s, v)
            fn(E).then_inc(sem, 1)
        self.streams[eng].append(run)
        self._commit(tok, reads, writes)
        self._tick()
        return tok

    def op_nosig(self, eng, fn, reads=(), writes=()):
        waits = self._deps(eng, reads, writes)

        def run(E, waits=waits, fn=fn):
            for (s, v) in waits:
                E.wait_ge(s, v)
            fn(E)
        self.streams[eng].append(run)

    def dma(self, qeng, fn, key, reads=(), writes=()):
        if key not in self.dsem:
            self.dsem[key] = [self.stack.enter_context(self.nc.semaphore("d_" + key)), 0]
        ent = self.dsem[key]
        sem = ent[0]
        waits = self._deps(qeng, reads, writes)
        if ent[1] > 0 and self.waited[qeng].get(id(sem), 0) < ent[1]:
            self.waited[qeng][id(sem)] = ent[1]
            waits = waits + [(sem, ent[1])]
        ent[1] += 16
        tok = ("dma:" + key, sem, ent[1])

        def run(E, waits=waits, fn=fn, sem=sem):
            for (s, v) in waits:
                E.wait_ge(s, v)
            fn(E).then_inc(sem, 16)
        self.streams[qeng].append(run)
        self._commit(tok, reads, writes)
        self._tick()
        return tok

    def barrier(self):
        self.drain()
        targets = [(self.esem[e], self.ecnt[e]) for e in ENGS if self.ecnt[e] > 0]
        targets += [(ent[0], ent[1]) for ent in self.dsem.values() if ent[1] > 0]
        for e in ENGS:
            w = self.waited[e]
            mine = []
            for (s, v) in targets:
                if s is self.esem[e]:
                    continue
                if w.get(id(s), 0) < v:
                    w[id(s)] = v
                    mine.append((s, v))

            def run(E, mine=mine):
                for (s, v) in mine:
                    E.wait_ge(s, v)
            self.streams[e].append(run)

    def emit(self):
        nc = self.nc
        with nc.Block() as block:
            @block.tensor
            def _(E):
                for f in self.streams["pe"]:
                    f(E)

            @block.vector
            def _(E):
                for f in self.streams["dve"]:
                    f(E)

            @block.scalar
            def _(E):
                for f in self.streams["act"]:
                    f(E)

            @block.gpsimd
            def _(E):
                for f in self.streams["pool"]:
                    f(E)

            @block.sync
            def _(E):
                for f in self.streams["sp"]:
                    f(E)
        self.streams = {e: [] for e in ENGS}


class Ring:
    def __init__(self, alloc, name, shape, dt, n):
        self.t = [alloc("%s%d" % (name, i), shape, dt) for i in range(n)]
        self.b = [Buf("%s%d" % (name, i)) for i in range(n)]
        self.i = 0

    def get(self):
        r = (self.t[self.i], self.b[self.i])
        self.i = (self.i + 1) % len(self.t)
        return r


class Ctx:
    def __init__(self, name):
        self.nc = bass.Bass("TRN2", target_bir_lowering=False)
        self.stack = contextlib.ExitStack()
        self.S = Sched(self.nc, self.stack)
        self.pstack = self.stack
        self.phase = 0
        self.nck = 0

    def dram_in(self, name, shape, dt):
        return self.nc.dram_tensor(name, list(shape), dt, kind="ExternalInput").ap()

    def dram_out(self, name, shape, dt):
        return self.nc.dram_tensor(name, list(shape), dt, kind="ExternalOutput").ap()

    def dram_tmp(self, name, shape, dt):
        return self.nc.dram_tensor(name, list(shape), dt).ap()

    def sb(self, name, shape, dt):
        return self.pstack.enter_context(self.nc.sbuf_tensor("sb%d_%s" % (self.phase, name), list(shape), dt))

    def ps(self, name, shape, dt):
        return self.pstack.enter_context(self.nc.psum_tensor("ps%d_%s" % (self.phase, name), list(shape), dt))

    def ckey(self):
        self.nck += 1
        return "const%d" % (self.nck % 6)

    def ldq(self):
        return "sp"

    def begin_phase(self):
        self.phase += 1
        self.pstack = contextlib.ExitStack()

    def end_phase(self):
        self.S.barrier()
        self.S.emit()
        self.pstack.close()
        self.pstack = self.stack

    def finish(self):
        self.S.barrier()
        self.S.emit()
        self.stack.close()
        return self.nc


def mm_chain(S, ps_ap, ps_buf, pairs, reads):
    n = len(pairs)
    for i, (l, r) in enumerate(pairs):
        fn = (lambda E, l=l, r=r, i=i: E.matmul(ps_ap, lhsT=l, rhs=r, start=(i == 0), stop=(i == n - 1)))
        if i < n - 1:
            S.op_nosig("pe", fn, reads=reads if i == 0 else (), writes=[ps_buf] if i == 0 else ())
        else:
            return S.op("pe", fn, reads=reads, writes=[ps_buf])


def load_consts(K, need_router=True):
    S = K.S
    cs = {}
    specs = [("ident_bf", [128, 128], BF16), ("ident_f", [128, 128], F32)]
    if need_router:
        specs += [("triu", [128, 128], BF16), ("ones_bf", [128, 128], BF16),
                  ("tril", [128, 128], F32), ("iota", [128, NE], F32), ("dump_p", [128, 1], F32)]
    for (nm, shp, dt) in specs:
        src = K.dram_in(nm, shp, dt)
        t = K.sb("c_" + nm, shp, dt)
        b = Buf("c_" + nm)
        S.dma("sp", lambda E, t=t, src=src: E.dma_start(out=t[:], in_=src), K.ckey(), writes=[b])
        cs[nm] = (t, b)
    return cs


def bcast_load(K, name, src_row, n, key=None):
    t = K.sb(name, [128, n], F32)
    b = Buf(name)
    key = key or K.ckey()
    K.S.dma("sp", lambda E: E.dma_start(out=t[:], in_=src_row.partition_broadcast(128)), key, writes=[b])
    return t, b


def layer_norm_tile(K, h, hB, g, gB, bta, bB, msk_ap, mB, out, outB, st):
    S = K.S
    stats, stB = st["stats"].get()
    mv, mvB = st["mv"].get()
    rstd, rsB = st["rstd"].get()
    for hh in range(2):
        S.op("dve", lambda E, hh=hh: E.bn_stats(out=stats[:, hh, :], in_=h[:, hh * 512:(hh + 1) * 512]),
             reads=[hB], writes=[stB])
    S.op("dve", lambda E: E.bn_aggr(out=mv[:], in_=stats[:].rearrange("p a b -> p (a b)")), reads=[stB], writes=[mvB])
    S.op("act", lambda E: E.activation(out=rstd[:, 0:1], in_=mv[:, 1:2], func=AF.Sqrt, bias=st["eps"][0][:, 0:1], scale=1.0),
         reads=[mvB, st["eps"][1]], writes=[rsB])
    S.op("dve", lambda E: E.reciprocal(out=rstd[:, 1:2], in_=rstd[:, 0:1]), reads=[rsB], writes=[rsB])
    if msk_ap is not None:
        S.op("dve", lambda E: E.tensor_tensor(out=rstd[:, 2:3], in0=rstd[:, 1:2], in1=msk_ap, op=ALU.mult),
             reads=[rsB, mB], writes=[rsB])
        rs_ap = rstd[:, 2:3]
    else:
        rs_ap = rstd[:, 1:2]
    t1, t1B = st["t4k"].get()
    S.op("dve", lambda E: E.tensor_scalar(out=t1[:], in0=h[:], scalar1=mv[:, 0:1], scalar2=rs_ap,
                                          op0=ALU.subtract, op1=ALU.mult), reads=[hB, mvB, rsB], writes=[t1B])
    S.op("dve", lambda E: E.tensor_tensor(out=t1[:], in0=t1[:], in1=g[:], op=ALU.mult), reads=[t1B, gB], writes=[t1B])
    if msk_ap is not None:
        S.op("dve", lambda E: E.scalar_tensor_tensor(out=out[:], in0=bta[:], scalar=msk_ap, in1=t1[:],
                                                     op0=ALU.mult, op1=ALU.add), reads=[bB, mB, t1B], writes=[outB])
    else:
        S.op("dve", lambda E: E.tensor_tensor(out=out[:], in0=t1[:], in1=bta[:], op=ALU.add), reads=[t1B, bB], writes=[outB])


def make_ln_state(K):
    st = {
        "stats": Ring(K.sb, "ln_stats", [128, 2, 6], F32, 2),
        "mv": Ring(K.sb, "ln_mv", [128, 2], F32, 2),
        "rstd": Ring(K.sb, "ln_rstd", [128, 4], F32, 2),
        "t4k": Ring(K.sb, "ln_t", [128, D], F32, 2),
    }
    eps = K.sb("ln_eps", [128, 1], F32)
    eB = Buf("ln_eps")
    K.S.op("pool", lambda E: E.memset(eps[:], EPS), writes=[eB])
    st["eps"] = (eps, eB)
    return st


def combine_phase(K, cs, lnst, x1p, yr, destp, gatep, lng_row, lnb_row, tmask, out_fn, big):
    S = K.S
    g, gB = bcast_load(K, "lnf_g", lng_row, D)
    bta, bB = bcast_load(K, "lnf_b", lnb_row, D)
    dst = K.sb("cmb_dest", [128, NTL * TOPK], U32)
    dB = Buf("cmb_dest")
    gt = K.sb("cmb_gate", [128, NTL * TOPK], F32)
    gtB = Buf("cmb_gate")
    S.dma("sp", lambda E: E.dma_start(out=dst[:], in_=destp), K.ckey(), writes=[dB])
    S.dma("sp", lambda E: E.dma_start(out=gt[:], in_=gatep), K.ckey(), writes=[gtB])
    outB = Buf("cmb_out_dram")
    yrB = Buf("yr_dram")
    for j in range(NTL):
        dest_ap = out_fn(j)
        if dest_ap is None:
            continue
        xt, xB = big.get()
        S.dma("sp", lambda E, xt=xt, j=j: E.dma_start(out=xt[:], in_=x1p[j * 128:(j + 1) * 128, :]), "cmb_x%d" % (j % 2), writes=[xB])
        acc, aB = big.get()
        S.op("dve", lambda E, acc=acc, xt=xt: E.tensor_scalar(out=acc[:], in0=xt[:], scalar1=ALPHA, scalar2=None, op0=ALU.mult),
             reads=[xB], writes=[aB])
        for k in range(TOPK):
            yk, yB = big.get()
            col = j * TOPK + k
            S.dma("pool", lambda E, yk=yk, col=col: E.indirect_dma_start(
                out=yk[:], out_offset=None, in_=yr,
                in_offset=bass.IndirectOffsetOnAxis(ap=dst[:, col:col + 1], axis=0)),
                "cmb_g%d" % k, reads=[dB, yrB], writes=[yB])
            S.op("dve", lambda E, yk=yk, acc=acc, col=col: E.scalar_tensor_tensor(
                out=acc[:], in0=yk[:], scalar=gt[:, col:col + 1], in1=acc[:], op0=ALU.mult, op1=ALU.add),
                reads=[yB, gtB, aB], writes=[aB])
        o, oB = big.get()
        layer_norm_tile(K, acc, aB, g, gB, bta, bB, tmask[0][:, j:j + 1], tmask[1], o, oB, lnst)
        S.dma("act", lambda E, o=o, dest_ap=dest_ap: E.dma_start(out=dest_ap, in_=o[:]), "cmb_o%d" % (j % 2), reads=[oB], writes=[outB])
    return outB


def emit_A(K, layer, first, io, cs, lnst, tm, tmB):
    even = (layer % 2 == 0)
    S = K.S
    nc = K.nc
    idb, idbB = cs["ident_bf"]
    idf, idfB = cs["ident_f"]
    big = Ring(K.sb, "big", [128, D], F32, 12)

    if first:
        x_in = io["x"]
        xin_B = Buf("x_in")
    else:
        x1p = io["x1p"]
        yr = io["yr"]
        destp = io["destp"]
        gatep = io["gatep"]
        lnfg = io["lnf_g_prev"]
        lnfb = io["lnf_b_prev"]
        x_in = io["x_mid"]
        xin_B = combine_phase(K, cs, lnst, x1p, yr, destp, gatep, lnfg, lnfb, (tm, tmB),
                              lambda j: x_in[j * 128:(j + 1) * 128, :], big)

    xs = io["xs"]
    x1_out = io["x1"]
    dest_out = io["dest"]
    gate_out = io["gate"]

    lnm_g, lnm_gB = bcast_load(K, "lnm_g", io["lnm_g"], D)
    lnm_b, lnm_bB = bcast_load(K, "lnm_b", io["lnm_b"], D)
    rw_d = io["router_w"]
    rw = K.sb("rw", [128, 8, NE], F32)
    rwB = Buf("rw")
    S.dma("sp", lambda E: E.dma_start(out=rw[:], in_=rw_d.rearrange("(k p) e -> p k e", p=128)), K.ckey(), writes=[rwB])
    rb, rbB = bcast_load(K, "rb", io["router_b"], NE)

    wout_d = io["w_out"]
    wout = K.sb("wout", [128, 8, D], BF16)
    woutB = Buf("wout")
    S.dma("pool", lambda E: E.dma_start(out=wout[:], in_=wout_d.rearrange("(k p) n -> p k n", p=128)), "wld", writes=[woutB])
    if even:
        win_d = io["w_in"]
        win = K.sb("win", [128, 8, 2560], BF16)
        winB = Buf("win")
        for hh in range(2):
            S.dma("pool", lambda E, hh=hh: E.dma_start(out=win[:, hh * 4:(hh + 1) * 4, :],
                                                      in_=win_d.rearrange("(k p) n -> p k n", p=128)[:, hh * 4:(hh + 1) * 4, :]),
                  "wld", writes=[winB])
        ang, angB = bcast_load(K, "a_norm_g", io["a_norm_g"], 512)
        ws_d = io["a_w_s"]
        wsf = K.sb("wsf", [128, 8, 128], F32)
        wsfB = Buf("wsf")
        S.dma("sp", lambda E: E.dma_start(out=wsf[:], in_=ws_d.rearrange("h t s -> t h s")), K.ckey(), writes=[wsfB])
        wsm = K.sb("wsm", [128, 8, 128], BF16)
        wsmB = Buf("wsm")
        tril, trilB = cs["tril"]
        for h in range(8):
            S.op("dve", lambda E, h=h: E.tensor_tensor(out=wsm[:, h, :], in0=wsf[:, h, :], in1=tril[:], op=ALU.mult),
                 reads=[wsfB, trilB], writes=[wsmB])
        wsT = K.sb("wsT", [128, 8, 128], BF16)
        wsTB = Buf("wsT")
        bs_d = io["a_b_s"]
        bsT = K.sb("bsT", [128, 4, 128], F32)
        bsTB = Buf("bsT")
        for h in range(8):
            c, half = h // 2, h % 2
            S.dma("sp", lambda E, h=h, c=c, half=half: E.dma_start(
                out=bsT[half * 64:(half + 1) * 64, c, :], in_=bs_d[h, :].partition_broadcast(64)), K.ckey(), writes=[bsTB])
        cw_d = io["b_conv_w"]
        cw = K.sb("cw", [128, 4, 3], F32)
        cwB = Buf("cw")
        for kk in range(3):
            for c in range(4):
                S.dma("sp", lambda E, kk=kk, c=c: E.dma_start(
                    out=cw[:, c, kk:kk + 1], in_=cw_d[kk, c * 128:(c + 1) * 128].rearrange("(p o) -> p o", o=1)),
                    K.ckey(), writes=[cwB])
    else:
        cin_d = io["w_in"]
        cin = K.sb("cin", [128, 8, D], BF16)
        cinB = Buf("cin")
        S.dma("pool", lambda E: E.dma_start(out=cin[:], in_=cin_d.rearrange("(k p) n -> p k n", p=128)), "wld", writes=[cinB])
        wg_d = io["c_w_grp"]
        wg = K.sb("wg", [128, 4, 2, 256], BF16)
        wgB = Buf("wg")
        for gi in range(4):
            S.dma("pool", lambda E, gi=gi: E.dma_start(out=wg[:, gi, :, :], in_=wg_d[gi].rearrange("(cc p) d -> p cc d", p=128)),
                  "wld", writes=[wgB])
        csc_d = io["c_scale"]
        csc = K.sb("csc", [128, 8], F32)
        cscB = Buf("csc")
        for c in range(8):
            S.dma("sp", lambda E, c=c: E.dma_start(out=csc[:, c:c + 1], in_=csc_d[c * 128:(c + 1) * 128].rearrange("(p o) -> p o", o=1)),
                  K.ckey(), writes=[cscB])
        invc_d = io["invc"]

    pT = K.ps("pT", [128, 8, 128], BF16)
    pTB = Buf("pT")
    pF = K.ps("pF", [128, 8, 128], F32)
    pFB = Buf("pF")
    pacc = Ring(K.ps, "pacc", [128, 512], F32, 4)
    prt = K.ps("prt", [128, 512], F32)
    prtB = Buf("prt")

    if even:
        for h in range(8):
            S.op("pe", lambda E, h=h: E.transpose(out=pT[:, h, :], in_=wsm[:, h, :], identity=idb[:]),
                 reads=[wsmB, idbB], writes=[pTB])
        S.op("dve", lambda E: E.tensor_copy(out=wsT[:], in_=pT[:]), reads=[pTB], writes=[wsTB])

    xb_r = Ring(K.sb, "xb", [128, D], BF16, 2)
    xT = K.sb("xT", [128, 8, NT], BF16)
    xTB = Buf("xT")
    yT = K.sb("yT", [128, 8, NT], BF16)
    yTB = Buf("yT")
    sm512 = Ring(K.sb, "sm512", [128, 512], F32, 3)
    fm = Ring(K.sb, "fm", [128, NT], F32, 6)
    if even:
        ug = K.sb("ug", [128, 4, NT], F32)
        ugB = [Buf("ug%d" % c) for c in range(4)]
        vnb_r = Ring(K.sb, "vnb", [128, 512], BF16, 2)
        zc = K.sb("zc", [128, 4, NT + 2], F32)
        zcB = [Buf("zc%d" % c) for c in range(4)]
        for c in range(4):
            S.op("pool", lambda E, c=c: E.memset(zc[:, c, 0:2], 0.0), writes=[zcB[c]])
        sv128 = Ring(K.sb, "sv128", [128, 128], F32, 3)
    else:
        ze = K.sb("ze", [128, 8, NT + 15], F32)
        zeB = [Buf("ze%d" % c) for c in range(8)]
        for c in range(8):
            S.op("pool", lambda E, c=c: E.memset(ze[:, c, 0:15], 0.0), writes=[zeB[c]])
        sAB = Ring(K.sb, "sAB", [128, NT + 15], F32, 3)
        pooled = K.sb("pooled", [128, 8, NT], BF16)
        pooledB = [Buf("pooled%d" % c) for c in range(8)]
        invc_r = Ring(K.sb, "invc", [128, 4, NT], F32, 2)
    x1b_r = Ring(K.sb, "x1b", [128, D], BF16, 3)
    x1T_r = Ring(K.sb, "x1T", [128, 8, 128], F32, 1)
    lg_r = Ring(K.sb, "lg", [128, NE], F32, 2)
    mx_r = Ring(K.sb, "mx", [128, 8], F32, 2)
    mi_r = Ring(K.sb, "mi", [128, 8], U32, 2)
    sm_r = Ring(K.sb, "smalls", [128, 16], F32, 2)
    cr_r = Ring(K.sb, "carry", [128, 16], F32, 2)
    junk_r = Ring(K.sb, "junk", [128, NE], F32, 2)
    Mb_r = Ring(K.sb, "Mb", [128, NE], BF16, 2)
    macc = K.sb("macc", [128, NE], BF16)
    maccB = Buf("macc")
    S.op("pool", lambda E: E.memset(macc[:], 0.0), writes=[maccB])
    dest_all = K.sb("dest_all", [128, NTL * TOPK], U32)
    destB = Buf("dest_all")
    gate_all = K.sb("gate_all", [128, NTL * TOPK], F32)
    gateB = Buf("gate_all")
    xsB = Buf("xs_dram")
    x1oB = Buf("x1_out")
    triu, triuB = cs["triu"]
    ones, onesB = cs["ones_bf"]
    iota, iotaB = cs["iota"]
    dump_p, dump_pB = cs["dump_p"]

    def route_tile(j, x1, x1B):
        x1b, x1bB = x1b_r.get()
        yield
        S.op("act", lambda E: E.copy(out=x1b[:], in_=x1[:]), reads=[x1B], writes=[x1bB])
        for k in range(8):
            yield
            S.op("pe", lambda E, k=k: E.transpose(out=pF[:, k, :], in_=x1[:, k * 128:(k + 1) * 128], identity=idf[:]),
                 reads=[x1B, idfB], writes=[pFB])
        x1T, x1TB = x1T_r.get()
        yield
        S.op("dve", lambda E: E.tensor_copy(out=x1T[:, 0:4, :], in_=pF[:, 0:4, :]), reads=[pFB], writes=[x1TB])
        yield
        S.op("act", lambda E: E.copy(out=x1T[:, 4:8, :], in_=pF[:, 4:8, :]), reads=[pFB], writes=[x1TB])
        pl, plB = prt, prtB
        yield
        mm_chain(S, pl[:, 0:NE], plB, [(x1T[:, k, :], rw[:, k, :]) for k in range(8)], reads=[x1TB, rwB])
        lg, lgB = lg_r.get()
        yield
        S.op("dve", lambda E: E.tensor_tensor(out=lg[:], in0=pl[:, 0:NE], in1=rb[:], op=ALU.add), reads=[plB, rbB], writes=[lgB])
        mx, mxB = mx_r.get()
        mi, miB = mi_r.get()
        yield
        S.op("dve", lambda E: E.max(out=mx[:], in_=lg[:]), reads=[lgB], writes=[mxB])
        yield
        S.op("dve", lambda E: E.max_index(out=mi[:], in_max=mx[:], in_values=lg[:]), reads=[lgB, mxB], writes=[miB])
        sm, smB = sm_r.get()
        yield
        S.op("dve", lambda E: E.tensor_scalar(out=sm[:, 0:1], in0=mx[:, 0:1], scalar1=-1.0, scalar2=None, op0=ALU.mult),
             reads=[mxB], writes=[smB])
        yield
        S.op("act", lambda E: E.activation(out=sm[:, 3:7], in_=mx[:, 0:4], func=AF.Exp, bias=sm[:, 0:1], scale=1.0,
                                           accum_out=sm[:, 1:2]), reads=[mxB, smB], writes=[smB])
        yield
        S.op("dve", lambda E: E.reciprocal(out=sm[:, 2:3], in_=sm[:, 1:2]), reads=[smB], writes=[smB])
        Mb, MbB = Mb_r.get()
        yield
        S.op("dve", lambda E: E.tensor_scalar(out=Mb[:], in0=lg[:], scalar1=mx[:, 3:4], scalar2=tm[:, j:j + 1],
                                              op0=ALU.is_ge, op1=ALU.mult), reads=[lgB, mxB, tmB], writes=[MbB])
        pr, prB = prt[:, NE:2 * NE], prtB
        yield
        mm_chain(S, pr[:, 0:NE], prB, [(triu[:], Mb[:]), (ones[:], macc[:])], reads=[triuB, onesB, MbB, maccB])
        yield
        S.op("dve", lambda E: E.tensor_tensor(out=macc[:], in0=macc[:], in1=Mb[:], op=ALU.add), reads=[maccB, MbB], writes=[maccB])
        yield
        S.op("dve", lambda E: E.tensor_copy(out=sm[:, 7:11], in_=mi[:, 0:4]), reads=[miB], writes=[smB])
        junk, junkB = junk_r.get()
        for k in range(TOPK):
            yield
            S.op("dve", lambda E, k=k: E.scalar_tensor_tensor(
                out=junk[:], in0=iota[:], scalar=sm[:, 7 + k:8 + k], in1=pr[:, 0:NE], op0=ALU.is_equal, op1=ALU.mult,
                accum_out=sm[:, 11 + k:12 + k]), reads=[iotaB, smB, prB], writes=[junkB, smB])
        sm2, sm2B = sm_r.get()
        yield
        S.op("dve", lambda E: E.scalar_tensor_tensor(out=sm2[:, 0:4], in0=sm[:, 7:11], scalar=float(C), in1=sm[:, 11:15],
                                                     op0=ALU.mult, op1=ALU.add), reads=[smB], writes=[sm2B])
        yield
        S.op("dve", lambda E: E.tensor_scalar(out=sm2[:, 4:8], in0=sm[:, 11:15], scalar1=float(C), scalar2=tm[:, j:j + 1],
                                              op0=ALU.is_lt, op1=ALU.mult), reads=[smB, tmB], writes=[sm2B])
        yield
        S.op("dve", lambda E: E.scalar_tensor_tensor(out=sm2[:, 8:12], in0=sm2[:, 0:4], scalar=dump_p[:, 0:1], in1=sm2[:, 4:8],
                                                     op0=ALU.subtract, op1=ALU.mult), reads=[sm2B, dump_pB], writes=[sm2B])
        yield
        S.op("dve", lambda E: E.tensor_scalar(out=sm2[:, 0:4], in0=sm2[:, 8:12], scalar1=dump_p[:, 0:1], scalar2=None, op0=ALU.add),
             reads=[sm2B, dump_pB], writes=[sm2B])
        yield
        S.op("dve", lambda E: E.tensor_copy(out=dest_all[:, j * TOPK:(j + 1) * TOPK], in_=sm2[:, 0:4]), reads=[sm2B], writes=[destB])
        yield
        S.op("dve", lambda E: E.scalar_tensor_tensor(out=gate_all[:, j * TOPK:(j + 1) * TOPK], in0=sm[:, 3:7], scalar=sm[:, 2:3],
                                                     in1=sm2[:, 4:8], op0=ALU.mult, op1=ALU.mult), reads=[smB, sm2B], writes=[gateB])
        for k in range(TOPK):
            col = j * TOPK + k
            yield
            S.dma("pool", lambda E, col=col, x1b=x1b: E.indirect_dma_start(
                out=xs, out_offset=bass.IndirectOffsetOnAxis(ap=dest_all[:, col:col + 1], axis=0), in_=x1b[:], in_offset=None),
                "scat%d" % k, reads=[x1bB, destB], writes=[xsB])

    def post_mix_tile(j, jj, xt, xtB, wbuf, wB):
        h, hB = big.get()
        for n in range(2):
            pm, pmB = pacc.get()
            mm_chain(S, pm[:], pmB, [(yT[:, c, jj * 128:(jj + 1) * 128], wbuf[:, c, n * 512:(n + 1) * 512]) for c in range(8)],
                     reads=[yTB, wB])
            S.op("dve", lambda E, n=n, pm=pm: E.scalar_tensor_tensor(
                out=h[:, n * 512:(n + 1) * 512], in0=xt[:, n * 512:(n + 1) * 512], scalar=ALPHA, in1=pm[:],
                op0=ALU.mult, op1=ALU.add), reads=[xtB, pmB], writes=[hB])
        x1, x1B = big.get()
        layer_norm_tile(K, h, hB, lnm_g, lnm_gB, lnm_b, lnm_bB, tm[:, j:j + 1], tmB, x1, x1B, lnst)
        S.dma("pool", lambda E, x1=x1, j=j: E.dma_start(out=x1_out[j * 128:(j + 1) * 128, :], in_=x1[:]), "x1o", reads=[x1B], writes=[x1oB])
        S.drain()
        S.bg = route_tile(j, x1, x1B)

    for g0 in range(0, NTL, GT):
        xts = []
        for jj in range(GT):
            j = g0 + jj
            xt, xtB = big.get()
            S.dma("sp", lambda E, xt=xt, j=j: E.dma_start(out=xt[:], in_=x_in[j * 128:(j + 1) * 128, :]), "xld%d" % jj,
                  reads=[xin_B], writes=[xtB])
            xts.append((xt, xtB))
            xb, xbB = xb_r.get()
            S.op("act", lambda E, xb=xb, xt=xt: E.copy(out=xb[:], in_=xt[:]), reads=[xtB], writes=[xbB])
            for k in range(8):
                S.op("pe", lambda E, k=k, xb=xb: E.transpose(out=pT[:, k, :], in_=xb[:, k * 128:(k + 1) * 128], identity=idb[:]),
                     reads=[xbB, idbB], writes=[pTB])
            S.op("dve", lambda E, jj=jj: E.tensor_copy(out=xT[:, :, jj * 128:(jj + 1) * 128], in_=pT[:]), reads=[pTB], writes=[xTB])

        if even:
            for c in range(4):
                pu, puB = pacc.get()
                mm_chain(S, pu[:, 0:NT], puB, [(win[:, k, c * 128:(c + 1) * 128], xT[:, k, :]) for k in range(8)], reads=[winB, xTB])
                S.op("act", lambda E, c=c, pu=pu: E.activation(out=ug[:, c, :], in_=pu[:, 0:NT], func=AF.Gelu), reads=[puB], writes=[ugB[c]])
            for jj in range(GT):
                pv, pvB = pacc.get()
                mm_chain(S, pv[:], pvB, [(xT[:, k, jj * 128:(jj + 1) * 128], win[:, k, 512:1024]) for k in range(8)], reads=[winB, xTB])
                vg, vgB = sm512.get()
                S.op("act", lambda E, vg=vg, pv=pv: E.activation(out=vg[:], in_=pv[:], func=AF.Gelu), reads=[pvB], writes=[vgB])
                stats, stB = lnst["stats"].get()
                mv, mvB = lnst["mv"].get()
                rstd, rsB = lnst["rstd"].get()
                S.op("dve", lambda E, vg=vg, stats=stats: E.bn_stats(out=stats[:, 0, :], in_=vg[:]), reads=[vgB], writes=[stB])
                S.op("dve", lambda E, stats=stats, mv=mv: E.bn_aggr(out=mv[:], in_=stats[:, 0, :]), reads=[stB], writes=[mvB])
                S.op("act", lambda E, rstd=rstd, mv=mv: E.activation(out=rstd[:, 0:1], in_=mv[:, 1:2], func=AF.Sqrt,
                                                                   bias=lnst["eps"][0][:, 0:1], scale=1.0),
                     reads=[mvB, lnst["eps"][1]], writes=[rsB])
                S.op("dve", lambda E, rstd=rstd: E.reciprocal(out=rstd[:, 1:2], in_=rstd[:, 0:1]), reads=[rsB], writes=[rsB])
                S.op("dve", lambda E, vg=vg, mv=mv, rstd=rstd: E.tensor_scalar(
                    out=vg[:], in0=vg[:], scalar1=mv[:, 0:1], scalar2=rstd[:, 1:2], op0=ALU.subtract, op1=ALU.mult),
                    reads=[vgB, mvB, rsB], writes=[vgB])
                vnb, vnbB = vnb_r.get()
                S.op("dve", lambda E, vnb=vnb, vg=vg: E.tensor_tensor(out=vnb[:], in0=vg[:], in1=ang[:], op=ALU.mult),
                     reads=[vgB, angB], writes=[vnbB])
                for c in range(4):
                    psv, psvB = pacc.get()
                    S.op_nosig("pe", lambda E, c=c, psv=psv, vnb=vnb: E.matmul(psv[:, 0:128], lhsT=vnb[:, c * 128:(c + 1) * 128],
                                                                              rhs=wsT[:, 2 * c, :], start=True, stop=True),
                               reads=[vnbB, wsTB], writes=[psvB])
                    S.op("pe", lambda E, c=c, psv=psv, vnb=vnb: E.matmul(psv[:, 128:256], lhsT=vnb[:, c * 128:(c + 1) * 128],
                                                                        rhs=wsT[:, 2 * c + 1, :], start=True, stop=True),
                         reads=[vnbB, wsTB], writes=[psvB])
                    sv, svB = sv128.get()
                    S.op("dve", lambda E, c=c, psv=psv, sv=sv: E.tensor_tensor(out=sv[0:64, :], in0=psv[0:64, 0:128], in1=bsT[0:64, c, :], op=ALU.add),
                         reads=[psvB, bsTB], writes=[svB])
                    S.op("dve", lambda E, c=c, psv=psv, sv=sv: E.tensor_tensor(out=sv[64:128, :], in0=psv[64:128, 128:256], in1=bsT[64:128, c, :], op=ALU.add),
                         reads=[psvB, bsTB], writes=[svB])
                    S.op("dve", lambda E, c=c, sv=sv, jj=jj: E.tensor_tensor(out=yT[:, c, jj * 128:(jj + 1) * 128], in0=sv[:],
                                                                            in1=ug[:, c, jj * 128:(jj + 1) * 128], op=ALU.mult),
                         reads=[svB, ugB[c]], writes=[yTB])
            for c in range(4):
                pxi, pxiB = pacc.get()
                mm_chain(S, pxi[:, 0:NT], pxiB, [(win[:, k, 2048 + c * 128:2048 + (c + 1) * 128], xT[:, k, :]) for k in range(8)], reads=[winB, xTB])
                xi, xiB = fm.get()
                S.op("act", lambda E, xi=xi, pxi=pxi: E.copy(out=xi[:], in_=pxi[:, 0:NT]), reads=[pxiB], writes=[xiB])
                pgc, pgcB = pacc.get()
                mm_chain(S, pgc[:, 0:NT], pgcB, [(win[:, k, 1536 + c * 128:1536 + (c + 1) * 128], xT[:, k, :]) for k in range(8)], reads=[winB, xTB])
                S.op("dve", lambda E, c=c, pgc=pgc, xi=xi: E.tensor_tensor(out=zc[:, c, 2:2 + NT], in0=pgc[:, 0:NT], in1=xi[:], op=ALU.mult),
                     reads=[pgcB, xiB], writes=[zcB[c]])
                o1, o1B = fm.get()
                S.op("dve", lambda E, c=c, o1=o1: E.tensor_scalar(out=o1[:], in0=zc[:, c, 0:NT], scalar1=cw[:, c, 0:1], scalar2=None, op0=ALU.mult),
                     reads=[zcB[c], cwB], writes=[o1B])
                S.op("dve", lambda E, c=c, o1=o1: E.scalar_tensor_tensor(out=o1[:], in0=zc[:, c, 1:NT + 1], scalar=cw[:, c, 1:2], in1=o1[:],
                                                                        op0=ALU.mult, op1=ALU.add), reads=[zcB[c], cwB, o1B], writes=[o1B])
                S.op("dve", lambda E, c=c, o1=o1: E.scalar_tensor_tensor(out=o1[:], in0=zc[:, c, 2:NT + 2], scalar=cw[:, c, 2:3], in1=o1[:],
                                                                        op0=ALU.mult, op1=ALU.add), reads=[zcB[c], cwB, o1B], writes=[o1B])
                cr, crB = cr_r.get()
                S.op("act", lambda E, c=c, cr=cr: E.copy(out=cr[:, 0:2], in_=zc[:, c, NT:NT + 2]), reads=[zcB[c]], writes=[crB])
                S.op("act", lambda E, c=c, cr=cr: E.copy(out=zc[:, c, 0:2], in_=cr[:, 0:2]), reads=[crB, zcB[c]], writes=[zcB[c]])
                pgb, pgbB = pacc.get()
                mm_chain(S, pgb[:, 0:NT], pgbB, [(win[:, k, 1024 + c * 128:1024 + (c + 1) * 128], xT[:, k, :]) for k in range(8)], reads=[winB, xTB])
                S.op("dve", lambda E, c=c, pgb=pgb, o1=o1: E.tensor_tensor(out=yT[:, 4 + c, :], in0=pgb[:, 0:NT], in1=o1[:], op=ALU.mult),
                     reads=[pgbB, o1B], writes=[yTB])
        else:
            ic, icB = invc_r.get()
            S.dma("sp", lambda E, ic=ic, g0=g0: E.dma_start(
                out=ic[:], in_=invc_d[:, g0 * 128:g0 * 128 + NT].partition_broadcast(128)), "invc", writes=[icB])
            for c in range(8):
                pz, pzB = pacc.get()
                mm_chain(S, pz[:, 0:NT], pzB, [(cin[:, k, c * 128:(c + 1) * 128], xT[:, k, :]) for k in range(8)], reads=[cinB, xTB])
                S.op("act", lambda E, c=c, pz=pz: E.copy(out=ze[:, c, 15:15 + NT], in_=pz[:, 0:NT]), reads=[pzB], writes=[zeB[c]])
                gi = c // 2
                src, srcB = ze[:, c, :], zeB[c]
                lo = 0
                for stp in range(gi + 1):
                    sh = 1 << stp
                    nlo = lo + sh
                    dstt, dstB = sAB.get()
                    S.op("dve", lambda E, src=src, dstt=dstt, nlo=nlo, sh=sh: E.tensor_tensor(
                        out=dstt[:, nlo:NT + 15], in0=src[:, nlo:NT + 15], in1=src[:, nlo - sh:NT + 15 - sh], op=ALU.add),
                        reads=[srcB], writes=[dstB])
                    src, srcB, lo = dstt, dstB, nlo
                pl_, plB_ = fm.get()
                S.op("dve", lambda E, src=src, pl_=pl_, gi=gi, ic=ic: E.tensor_tensor(out=pl_[:], in0=src[:, 15:15 + NT], in1=ic[:, gi, :], op=ALU.mult),
                     reads=[srcB, icB], writes=[plB_])
                S.op("dve", lambda E, c=c, pl_=pl_: E.tensor_tensor(out=pooled[:, c, :], in0=pl_[:], in1=ze[:, c, 15:15 + NT], op=ALU.subtract),
                     reads=[plB_, zeB[c]], writes=[pooledB[c]])
                cr, crB = cr_r.get()
                S.op("act", lambda E, c=c, cr=cr: E.copy(out=cr[:, 0:15], in_=ze[:, c, NT:NT + 15]), reads=[zeB[c]], writes=[crB])
                S.op("act", lambda E, c=c, cr=cr: E.copy(out=ze[:, c, 0:15], in_=cr[:, 0:15]), reads=[crB, zeB[c], pooledB[c]], writes=[zeB[c]])
            for c2 in range(8):
                gi = c2 // 2
                pmx, pmxB = pacc.get()
                mm_chain(S, pmx[:, 0:NT], pmxB, [(wg[:, gi, cc, (c2 % 2) * 128:(c2 % 2 + 1) * 128], pooled[:, 2 * gi + cc, :]) for cc in range(2)],
                         reads=[wgB, pooledB[2 * gi], pooledB[2 * gi + 1]])
                S.op("act", lambda E, c2=c2, pmx=pmx: E.activation(out=yT[:, c2, :], in_=pmx[:, 0:NT], func=AF.Copy, scale=csc[:, c2:c2 + 1]),
                     reads=[pmxB, cscB], writes=[yTB])

        for jj in range(GT):
            post_mix_tile(g0 + jj, jj, xts[jj][0], xts[jj][1], wout, woutB)

    S.drain()
    S.dma("sp", lambda E: E.dma_start(out=dest_out, in_=dest_all[:]), "fin", reads=[destB], writes=[Buf("o1")])
    S.dma("sp", lambda E: E.dma_start(out=gate_out, in_=gate_all[:]), "fin", reads=[gateB], writes=[Buf("o2")])


A_IN_COMMON = [("tmask", [128, NTL], F32), ("lnm_g", [D], F32), ("lnm_b", [D], F32), ("router_w", [D, NE], F32),
               ("router_b", [NE], F32), ("w_out", [D, D], F32)]
A_IN_EVEN = [("w_in", [D, 2560], F32), ("a_norm_g", [512], F32), ("a_w_s", [8, 128, 128], F32), ("a_b_s", [8, 128], F32),
             ("b_conv_w", [3, 512], F32)]
A_IN_ODD = [("w_in", [D, D], F32), ("c_w_grp", [4, 256, 256], F32), ("c_scale", [D], F32), ("invc", [4, T], F32)]
A_IN_PREV = [("x1p", [T, D], F32), ("yr", [XS_ROWS, D], F32), ("destp", [128, NTL * TOPK], U32),
             ("gatep", [128, NTL * TOPK], F32), ("lnf_g_prev", [D], F32), ("lnf_b_prev", [D], F32)]
A_OUT = [("xs", [XS_ROWS, D], BF16), ("x1", [T, D], F32), ("dest", [128, NTL * TOPK], U32), ("gate", [128, NTL * TOPK], F32)]


def load_tmask(K, tmask_d):
    tm = K.sb("tmask_sb", [128, NTL], F32)
    tmB = Buf("tmask")
    K.S.dma("sp", lambda E: E.dma_start(out=tm[:], in_=tmask_d), K.ckey(), writes=[tmB])
    return tm, tmB


def build_A(layer, first):
    K = Ctx("A%d" % layer)
    cs = load_consts(K)
    lnst = make_ln_state(K)
    io = {}
    specs = A_IN_COMMON + (A_IN_EVEN if layer % 2 == 0 else A_IN_ODD) + ([("x", [T, D], F32)] if first else A_IN_PREV)
    for (nm, shp, dt) in specs:
        io[nm] = K.dram_in(nm, shp, dt)
    for (nm, shp, dt) in A_OUT:
        io[nm] = K.dram_out(nm, shp, dt)
    if not first:
        io["x_mid"] = K.dram_tmp("x_mid", [T, D], F32)
    tm, tmB = load_tmask(K, io["tmask"])
    emit_A(K, layer, first, io, cs, lnst, tm, tmB)
    return K.finish()


def expert_phase(K, cs, xr, ys, wup_d, bup_d, wdn_d, bdn_d, xrB, ysB, n_exp, tile_fn, gw):
    S = K.S
    idb, idbB = cs["ident_bf"]
    NW = gw * 128
    wu_r = Ring(K.sb, "wu", [128, 8, 2048], BF16, 2)
    wd_r = Ring(K.sb, "wd", [128, 8, D], BF16, 2)
    bup_r = Ring(K.sb, "bup", [128, 16], F32, 2)
    bdn_r = Ring(K.sb, "bdn", [128, D], F32, 2)
    xg_r = Ring(K.sb, "xg", [128, D], BF16, 6)
    xgT_r = Ring(K.sb, "xgT", [128, 8, NW], BF16, 2)
    actT_r = Ring(K.sb, "actT", [128, 8, NW], BF16, 2)
    g_r = Ring(K.sb, "sw_g", [128, NW], F32, 3)
    sg_r = Ring(K.sb, "sw_s", [128, NW], F32, 3)
    l_r = Ring(K.sb, "sw_l", [128, NW], F32, 3)
    ysb_r = Ring(K.sb, "ysb", [128, D], F32, 2)
    pT_r = Ring(K.ps, "pTe", [128, 8, 128], BF16, 2)
    pacc = Ring(K.ps, "pacce", [128, 512], F32, 6)

    def load_wu(le):
        wu, wuB = wu_r.get()
        for hh in range(2):
            S.dma("pool", lambda E, hh=hh, wu=wu: E.dma_start(
                out=wu[:, hh * 4:(hh + 1) * 4, :], in_=wup_d[le].rearrange("(k p) n -> p k n", p=128)[:, hh * 4:(hh + 1) * 4, :]),
                "wu%d" % hh, writes=[wuB])
        return (wu, wuB)

    def load_rest(le):
        wd, wdB = wd_r.get()
        bup, bupB = bup_r.get()
        bdn, bdnB = bdn_r.get()
        S.dma("pool", lambda E, wd=wd: E.dma_start(out=wd[:], in_=wdn_d[le].rearrange("(k p) n -> p k n", p=128)), "wd", writes=[wdB])
        S.dma("sp", lambda E, bup=bup: E.dma_start(out=bup[:], in_=bup_d[le].rearrange("(f p) -> p f", p=128),
                                                  allow_slow_non_contiguous=True), "bup", writes=[bupB])
        S.dma("sp", lambda E, bdn=bdn: E.dma_start(out=bdn[:], in_=bdn_d[le].partition_broadcast(128)), "bdn", writes=[bdnB])
        return (wd, wdB, bup, bupB, bdn, bdnB)

    groups = []
    for le in range(n_exp):
        tiles = tile_fn(le)
        for q0 in range(0, len(tiles), gw):
            groups.append((le, tiles[q0:q0 + gw]))

    def stage_T(gi):
        le, grp = groups[gi]
        xgT, xgTB = xgT_r.get()
        for q, r0 in enumerate(grp):
            xg, xgB = xg_r.get()
            S.dma("sp", lambda E, xg=xg, r0=r0: E.dma_start(out=xg[:], in_=xr[r0:r0 + 128, :]), "xg%d" % q, reads=[xrB], writes=[xgB])
            pT, pTB = pT_r.get()
            for k in range(8):
                fn = lambda E, k=k, xg=xg, pT=pT: E.transpose(out=pT[:, k, :], in_=xg[:, k * 128:(k + 1) * 128], identity=idb[:])
                if k < 7:
                    S.op_nosig("pe", fn, reads=[xgB, idbB] if k == 0 else (), writes=[pTB] if k == 0 else ())
                else:
                    S.op("pe", fn, reads=[xgB, idbB], writes=[pTB])
            S.op("act", lambda E, q=q, pT=pT, xgT=xgT: E.copy(out=xgT[:, :, q * 128:(q + 1) * 128], in_=pT[:]), reads=[pTB], writes=[xgTB])
        return xgT, xgTB

    wucache = {}
    wrcache = {}

    def get_wu(le):
        if le not in wucache:
            wucache[le] = load_wu(le)
        return wucache[le]

    def get_rest(le):
        if le not in wrcache:
            wrcache[le] = load_rest(le)
        return wrcache[le]

    def weights(le):
        wu, wuB = get_wu(le)
        wd, wdB, bup, bupB, bdn, bdnB = get_rest(le)
        return (wu, wuB, wd, wdB, bup, bupB, bdn, bdnB)

    weights(0)
    if n_exp > 1:
        get_wu(1)
    cur = stage_T(0)
    for gi, (le, grp) in enumerate(groups):
        wu, wuB, wd, wdB, bup, bupB, bdn, bdnB = weights(le)
        if le + 1 < n_exp and (gi == 0 or groups[gi - 1][0] != le):
            get_rest(le + 1)
        xgT, xgTB = cur
        nw = len(grp) * 128
        actT, actTB = actT_r.get()
        pend = None
        for fc in range(9):
            if fc < 8:
                phg, phgB = pacc.get()
                mm_chain(S, phg[:, 0:nw], phgB, [(wu[:, k, fc * 128:(fc + 1) * 128], xgT[:, k, 0:nw]) for k in range(8)], reads=[wuB, xgTB])
                phl, phlB = pacc.get()
                mm_chain(S, phl[:, 0:nw], phlB, [(wu[:, k, (fc + 8) * 128:(fc + 9) * 128], xgT[:, k, 0:nw]) for k in range(8)], reads=[wuB, xgTB])
                g, gB = g_r.get()
                S.op("dve", lambda E, g=g, phg=phg, fc=fc, bup=bup: E.tensor_scalar(out=g[:, 0:nw], in0=phg[:, 0:nw], scalar1=bup[:, fc:fc + 1], scalar2=7.0,
                                                                              op0=ALU.add, op1=ALU.min), reads=[phgB, bupB], writes=[gB])
                sg, sgB = sg_r.get()
                S.op("act", lambda E, g=g, sg=sg: E.activation(out=sg[:, 0:nw], in_=g[:, 0:nw], func=AF.Sigmoid, scale=1.702), reads=[gB], writes=[sgB])
                l1, l1B = l_r.get()
                S.op("dve", lambda E, l1=l1, phl=phl, fc=fc, bup=bup: E.tensor_scalar(out=l1[:, 0:nw], in0=phl[:, 0:nw], scalar1=bup[:, fc + 8:fc + 9], scalar2=7.0,
                                                                                op0=ALU.add, op1=ALU.min), reads=[phlB, bupB], writes=[l1B])
                S.op("dve", lambda E, l1=l1: E.tensor_scalar(out=l1[:, 0:nw], in0=l1[:, 0:nw], scalar1=-7.0, scalar2=1.0, op0=ALU.max, op1=ALU.add),
                     reads=[l1B], writes=[l1B])
                new_pend = (fc, g, gB, sg, sgB, l1, l1B)
            else:
                new_pend = None
            if pend is not None:
                pfc, pg, pgB, psg, psgB, pl1, pl1B = pend
                S.op("dve", lambda E, pg=pg, psg=psg: E.tensor_tensor(out=psg[:, 0:nw], in0=pg[:, 0:nw], in1=psg[:, 0:nw], op=ALU.mult),
                     reads=[pgB, psgB], writes=[psgB])
                S.op("dve", lambda E, pl1=pl1, psg=psg, pfc=pfc, actT=actT: E.tensor_tensor(out=actT[:, pfc, 0:nw], in0=pl1[:, 0:nw], in1=psg[:, 0:nw], op=ALU.mult),
                     reads=[pl1B, psgB], writes=[actTB])
            pend = new_pend
        last_of_expert = (gi + 1 == len(groups)) or (groups[gi + 1][0] != le)
        if last_of_expert and le + 2 < n_exp:
            get_wu(le + 2)
        if gi + 1 < len(groups):
            nle = groups[gi + 1][0]
            weights(nle)
            cur = stage_T(gi + 1)
        for q, r0 in enumerate(grp):
            ysb, ysbB = ysb_r.get()
            for n in range(2):
                py, pyB = pacc.get()
                mm_chain(S, py[:], pyB, [(actT[:, fc, q * 128:(q + 1) * 128], wd[:, fc, n * 512:(n + 1) * 512]) for fc in range(8)],
                         reads=[actTB, wdB])
                S.op("dve", lambda E, n=n, py=py, ysb=ysb, bdn=bdn: E.tensor_tensor(
                    out=ysb[:, n * 512:(n + 1) * 512], in0=py[:], in1=bdn[:, n * 512:(n + 1) * 512], op=ALU.add),
                    reads=[pyB, bdnB], writes=[ysbB])
            S.dma("sp", lambda E, ysb=ysb, r0=r0: E.dma_start(out=ys[r0:r0 + 128, :], in_=ysb[:]), "yso%d" % (q % 2), reads=[ysbB], writes=[ysB])


def build_B():
    K = Ctx("B")
    cs = load_consts(K, need_router=False)
    xr = K.dram_in("xr", [NSLOT, D], BF16)
    wup_d = K.dram_in("w_up", [EPC, D, 2048], F32)
    bup_d = K.dram_in("b_up", [EPC, 2048], F32)
    wdn_d = K.dram_in("w_down", [EPC, D, D], F32)
    bdn_d = K.dram_in("b_down", [EPC, D], F32)
    ys = K.dram_out("ys", [NSLOT, D], F32)
    expert_phase(K, cs, xr, ys, wup_d, bup_d, wdn_d, bdn_d, Buf("xr"), Buf("ys"), EPC,
                 lambda le: [(c * EPC + le) * C + s_ * 128 for c in range(NCORES) for s_ in range(C // 128)], 4)
    return K.finish()


def build_C():
    K = Ctx("C")
    S = K.S
    cs = load_consts(K, need_router=False)
    lnst = make_ln_state(K)
    big = Ring(K.sb, "big", [128, D], F32, 8)
    tmask_d = K.dram_in("tmask", [128, NTL], F32)
    tm = K.sb("tmask_sb", [128, NTL], F32)
    tmB = Buf("tmask")
    S.dma("sp", lambda E: E.dma_start(out=tm[:], in_=tmask_d), K.ckey(), writes=[tmB])
    x1p = K.dram_in("x1p", [T, D], F32)
    yr = K.dram_in("yr", [XS_ROWS, D], F32)
    destp = K.dram_in("destp", [128, NTL * TOPK], U32)
    gatep = K.dram_in("gatep", [128, NTL * TOPK], F32)
    lnfg = K.dram_in("lnf_g_prev", [D], F32)
    lnfb = K.dram_in("lnf_b_prev", [D], F32)
    out = K.dram_out("out", [TOK, D], F32)
    nh = HALO // 128
    combine_phase(K, cs, lnst, x1p, yr, destp, gatep, lnfg, lnfb, (tm, tmB),
                  lambda j: (out[(j - nh) * 128:(j - nh + 1) * 128, :] if j >= nh else None), big)
    return K.finish()


def build_fused(depth=DEPTH):
    K = Ctx("F")
    S = K.S
    cs = load_consts(K)
    lnst = make_ln_state(K)
    tm, tmB = load_tmask(K, K.dram_in("tmask", [128, NTL], F32))
    x = K.dram_in("x", [T, D], F32)
    invc = K.dram_in("invc", [4, T], F32)
    out = K.dram_out("out", [TOK, D], F32)
    xs = K.dram_tmp("xs_i", [XS_ROWS, D], BF16)
    ys = K.dram_tmp("ys_i", [XS_ROWS, D], F32)
    x1d = [K.dram_tmp("x1_i%d" % i, [T, D], F32) for i in range(2)]
    xmid = K.dram_tmp("xmid_i", [T, D], F32)
    dsd = [K.dram_tmp("dest_i%d" % i, [128, NTL * TOPK], U32) for i in range(2)]
    gtd = [K.dram_tmp("gate_i%d" % i, [128, NTL * TOPK], F32) for i in range(2)]
    W = []
    for l in range(depth):
        w = {}
        specs = [s_ for s_ in A_IN_COMMON if s_[0] != "tmask"] + (A_IN_EVEN if l % 2 == 0 else [s_ for s_ in A_IN_ODD if s_[0] != "invc"])
        for (nm, shp, dt) in specs:
            w[nm] = K.dram_in("%s_%d" % (nm, l), shp, dt)
        w["lnf_g"] = K.dram_in("lnf_g_%d" % l, [D], F32)
        w["lnf_b"] = K.dram_in("lnf_b_%d" % l, [D], F32)
        w["w_up"] = K.dram_in("w_up_%d" % l, [NE, D, 2048], F32)
        w["b_up"] = K.dram_in("b_up_%d" % l, [NE, 2048], F32)
        w["w_down"] = K.dram_in("w_down_%d" % l, [NE, D, D], F32)
        w["b_down"] = K.dram_in("b_down_%d" % l, [NE, D], F32)
        W.append(w)

    K.begin_phase()
    zt = K.sb("zeros", [128, 4096], F32)
    zB = Buf("zeros")
    S.op("pool", lambda E: E.memset(zt[:], 0.0), writes=[zB])
    ztb = zt[:].bitcast(BF16)
    scrB = Buf("scratch")
    for i, r0 in enumerate(range(0, XS_ROWS - 128, 1024)):
        nr = min(1024, XS_ROWS - r0)
        S.dma("sp", lambda E, r0=r0, nr=nr: E.dma_start(out=xs[r0:r0 + nr, :].rearrange("(p a) n -> p (a n)", p=128),
                                                       in_=ztb[:, 0:(nr // 128) * D]), "z%d" % (i % 4), reads=[zB], writes=[scrB])
    S.dma("sp", lambda E: E.dma_start(out=xs[NSLOT:XS_ROWS, :], in_=ztb[:, 0:D]), "z0", reads=[zB], writes=[scrB])
    S.dma("sp", lambda E: E.dma_start(out=ys[NSLOT:XS_ROWS, :], in_=zt[:, 0:D]), "z1", reads=[zB], writes=[scrB])
    K.end_phase()

    for l in range(depth):
        io = dict(W[l])
        io["invc"] = invc
        io["xs"] = xs
        io["x1"] = x1d[l % 2]
        io["dest"] = dsd[l % 2]
        io["gate"] = gtd[l % 2]
        if l == 0:
            io["x"] = x
        else:
            K.begin_phase()
            cbig = Ring(K.sb, "cbig", [128, D], F32, 28)
            combine_phase(K, cs, lnst, x1d[(l - 1) % 2], ys, dsd[(l - 1) % 2], gtd[(l - 1) % 2],
                          W[l - 1]["lnf_g"], W[l - 1]["lnf_b"], (tm, tmB),
                          lambda j: xmid[j * 128:(j + 1) * 128, :], cbig)
            K.end_phase()
            io["x"] = xmid
        K.begin_phase()
        emit_A(K, l, True, io, cs, lnst, tm, tmB)
        K.end_phase()
        K.begin_phase()
        expert_phase(K, cs, xs, ys, W[l]["w_up"], W[l]["b_up"], W[l]["w_down"], W[l]["b_down"], Buf("xr"), Buf("ys"), NE,
                     lambda le: [le * C + s_ * 128 for s_ in range(C // 128)], C // 128)
        K.end_phase()
    K.begin_phase()
    big = Ring(K.sb, "bigc", [128, D], F32, 28)
    nh = HALO // 128
    lp = depth - 1
    combine_phase(K, cs, lnst, x1d[lp % 2], ys, dsd[lp % 2], gtd[lp % 2], W[lp]["lnf_g"], W[lp]["lnf_b"], (tm, tmB),
                  lambda j: (out[(j - nh) * 128:(j - nh + 1) * 128, :] if j >= nh else None), big)
    K.end_phase()
    return K.finish()


def _consts():
    return {
        "ident_bf": np.eye(128).astype(ml_dtypes.bfloat16),
        "ident_f": np.eye(128).astype(np.float32),
        "triu": np.triu(np.ones((128, 128), np.float32), 1).astype(ml_dtypes.bfloat16),
        "ones_bf": np.ones((128, 128), np.float32).astype(ml_dtypes.bfloat16),
        "tril": np.tril(np.ones((128, 128), np.float32)),
        "iota": np.tile(np.arange(NE, dtype=np.float32), (128, 1)),
        "dump_p": (DUMP + np.arange(128, dtype=np.float32)).reshape(128, 1),
    }


def _core_consts(c):
    start = c * TOK - HALO
    gpos = start + np.arange(T)
    tmask = np.ascontiguousarray((gpos >= 0).astype(np.float32).reshape(NTL, 128).T)
    invc = np.stack([1.0 / np.minimum(np.maximum(gpos, 0) + 1, w) for w in (2, 4, 8, 16)]).astype(np.float32)
    return tmask, invc


_PROGS = {}


def _prog(key, fn):
    if key not in _PROGS:
        _PROGS[key] = fn()
    return _PROGS[key]


def kernel_unfused(x, ab_w_in, a_norm_g, a_w_s, a_b_s, b_conv_w, ab_w_out, c_w_in, c_w_grp, c_scale, c_w_out,
           ln_mix_g, ln_mix_b, router_w, router_b, moe_w_up, moe_b_up, moe_w_down, moe_b_down, ln_ffn_g, ln_ffn_b):
    f32 = lambda a: np.ascontiguousarray(np.asarray(a, dtype=np.float32))
    x = f32(x)[0]
    cst = _consts()
    cc = [_core_consts(c) for c in range(NCORES)]
    cores = list(range(NCORES))
    rout_keys = ("ident_bf", "ident_f", "triu", "ones_bf", "tril", "iota", "dump_p")
    state = None
    for layer in range(DEPTH):
        i = layer // 2
        first = (layer == 0)
        nc = _prog(("A", layer % 2, first), lambda: build_A(layer, first))
        maps = []
        for c in cores:
            m = {k: cst[k] for k in rout_keys}
            m["tmask"] = cc[c][0]
            m["lnm_g"] = f32(ln_mix_g[layer])
            m["lnm_b"] = f32(ln_mix_b[layer])
            m["router_w"] = f32(router_w[layer])
            m["router_b"] = f32(router_b[layer])
            if layer % 2 == 0:
                m.update({"w_in": f32(ab_w_in[i]), "w_out": f32(ab_w_out[i]), "a_norm_g": f32(a_norm_g[i]),
                          "a_w_s": f32(a_w_s[i]), "a_b_s": f32(a_b_s[i]), "b_conv_w": f32(b_conv_w[i])})
            else:
                m.update({"w_in": f32(c_w_in[i]), "w_out": f32(c_w_out[i]), "c_w_grp": f32(c_w_grp[i]),
                          "c_scale": f32(c_scale[i]), "invc": cc[c][1]})
            if first:
                start = c * TOK - HALO
                lo = max(start, 0)
                xl = np.zeros((T, D), np.float32)
                xl[lo - start:] = x[lo:start + T]
                m["x"] = xl
            else:
                m.update({"x1p": state[c]["x1"], "yr": state[c]["yr"], "destp": state[c]["dest"], "gatep": state[c]["gate"],
                          "lnf_g_prev": f32(ln_ffn_g[layer - 1]), "lnf_b_prev": f32(ln_ffn_b[layer - 1])})
            maps.append(m)
        resA = run_bass_kernel_spmd(nc, maps, core_ids=cores).results
        ncb = _prog(("B",), build_B)
        mapsB = []
        for d in cores:
            xr = np.concatenate([resA[c]["xs"][d * EPC * C:(d + 1) * EPC * C] for c in cores], axis=0)
            mapsB.append({"ident_bf": cst["ident_bf"], "ident_f": cst["ident_f"], "xr": xr,
                          "w_up": f32(moe_w_up[layer, d * EPC:(d + 1) * EPC]), "b_up": f32(moe_b_up[layer, d * EPC:(d + 1) * EPC]),
                          "w_down": f32(moe_w_down[layer, d * EPC:(d + 1) * EPC]), "b_down": f32(moe_b_down[layer, d * EPC:(d + 1) * EPC])})
        resB = run_bass_kernel_spmd(ncb, mapsB, core_ids=cores).results
        state = []
        for c in cores:
            yr = np.zeros((XS_ROWS, D), np.float32)
            for d in cores:
                yr[d * EPC * C:(d + 1) * EPC * C] = resB[d]["ys"][c * EPC * C:(c + 1) * EPC * C]
            state.append({"x1": resA[c]["x1"], "dest": resA[c]["dest"], "gate": resA[c]["gate"], "yr": yr})
        del resA, resB
    ncc = _prog(("C",), build_C)
    mapsC = []
    for c in cores:
        mapsC.append({"ident_bf": cst["ident_bf"], "ident_f": cst["ident_f"], "tmask": cc[c][0],
                      "x1p": state[c]["x1"], "yr": state[c]["yr"], "destp": state[c]["dest"], "gatep": state[c]["gate"],
                      "lnf_g_prev": f32(ln_ffn_g[DEPTH - 1]), "lnf_b_prev": f32(ln_ffn_b[DEPTH - 1])})
    resC = run_bass_kernel_spmd(ncc, mapsC, core_ids=cores).results
    out = np.concatenate([resC[c]["out"] for c in cores], axis=0)
    return out.reshape(1, SEQ, D).astype(np.float32)


def kernel(x, ab_w_in, a_norm_g, a_w_s, a_b_s, b_conv_w, ab_w_out, c_w_in, c_w_grp, c_scale, c_w_out,
           ln_mix_g, ln_mix_b, router_w, router_b, moe_w_up, moe_b_up, moe_w_down, moe_b_down, ln_ffn_g, ln_ffn_b):
    f32 = lambda a: np.ascontiguousarray(np.asarray(a, dtype=np.float32))
    x = f32(x)[0]
    cst = _consts()
    shared = {}
    for l in range(DEPTH):
        i = l // 2
        shared.update({"lnm_g_%d" % l: f32(ln_mix_g[l]), "lnm_b_%d" % l: f32(ln_mix_b[l]), "router_w_%d" % l: f32(router_w[l]),
                       "router_b_%d" % l: f32(router_b[l]), "lnf_g_%d" % l: f32(ln_ffn_g[l]), "lnf_b_%d" % l: f32(ln_ffn_b[l]),
                       "w_up_%d" % l: f32(moe_w_up[l]), "b_up_%d" % l: f32(moe_b_up[l]),
                       "w_down_%d" % l: f32(moe_w_down[l]), "b_down_%d" % l: f32(moe_b_down[l])})
        if l % 2 == 0:
            shared.update({"w_in_%d" % l: f32(ab_w_in[i]), "w_out_%d" % l: f32(ab_w_out[i]), "a_norm_g_%d" % l: f32(a_norm_g[i]),
                           "a_w_s_%d" % l: f32(a_w_s[i]), "a_b_s_%d" % l: f32(a_b_s[i]), "b_conv_w_%d" % l: f32(b_conv_w[i])})
        else:
            shared.update({"w_in_%d" % l: f32(c_w_in[i]), "w_out_%d" % l: f32(c_w_out[i]), "c_w_grp_%d" % l: f32(c_w_grp[i]),
                           "c_scale_%d" % l: f32(c_scale[i])})
    maps = []
    for c in range(NCORES):
        tmask, invc = _core_consts(c)
        start = c * TOK - HALO
        lo = max(start, 0)
        xl = np.zeros((T, D), np.float32)
        xl[lo - start:] = x[lo:start + T]
        m = dict(cst)
        m.update(shared)
        m.update({"x": xl, "tmask": tmask, "invc": invc})
        maps.append(m)
    nc = _prog(("F",), build_fused)
    res = run_bass_kernel_spmd(nc, maps, core_ids=list(range(NCORES))).results
    out = np.concatenate([res[c]["out"] for c in range(NCORES)], axis=0)
    return out.reshape(1, SEQ, D).astype(np.float32)
```
